# Optimizing a Trainium2 kernel written in Bass

```python
import math
import jax, jax.numpy as jnp
from jax import lax
import numpy as np

D_MODEL = 2048
BATCH = 8
SEQ = 2048
DEPTH = 1

HEAD_DIM = 64
ROPE_THETA = 10000.0
RMS_EPS = 1e-6
NEG_INF = -1e30
SWA_Q_HEADS = 16
SWA_KV_HEADS = 2
SWA_GROUP = SWA_Q_HEADS // SWA_KV_HEADS
WINDOW = 128
SWA_BLOCK = WINDOW
DIFF_HEADS = 8
DIFF_V_DIM = 2 * HEAD_DIM
Q_BLOCK = 128
SWA_Q_COLS = SWA_Q_HEADS * HEAD_DIM
SWA_KV_COLS = SWA_KV_HEADS * HEAD_DIM
DIFF_QK_COLS = DIFF_HEADS * 2 * HEAD_DIM
DIFF_V_COLS = DIFF_HEADS * DIFF_V_DIM
GATE_COLS = 2 * D_MODEL
IN_COLS = SWA_Q_COLS + 2 * SWA_KV_COLS + 2 * DIFF_QK_COLS + DIFF_V_COLS + GATE_COLS
N_GROUPS = 8
EXPERTS_PER_GROUP = 8
N_EXPERTS = N_GROUPS * EXPERTS_PER_GROUP
TOP_K = 2
D_EXPERT = D_MODEL // 2
MOE_BLOCK = 128

kernel_name = "hybrid_swa_sink_diffattn_hiermoe_adaln"


def rmsnorm(x, g):
    xf = x.astype(jnp.float32)
    y = xf * lax.rsqrt(jnp.mean(xf * xf, axis=-1, keepdims=True) + RMS_EPS)
    return (y * g.astype(jnp.float32)).astype(x.dtype)


def rope_tables(positions):
    inv_freq = ROPE_THETA ** (-jnp.arange(0, HEAD_DIM, 2, dtype=jnp.float32) / HEAD_DIM)
    ang = positions.astype(jnp.float32)[..., None] * inv_freq
    return jnp.cos(ang), jnp.sin(ang)


def apply_rope(x, cos, sin):
    shp = cos.shape[:2] + (1,) * (x.ndim - 3) + cos.shape[-1:]
    cos, sin = cos.reshape(shp), sin.reshape(shp)
    xf = x.astype(jnp.float32)
    x1, x2 = jnp.split(xf, 2, axis=-1)
    return jnp.concatenate([x1 * cos - x2 * sin, x2 * cos + x1 * sin], axis=-1).astype(x.dtype)


def swa_sink_attention(q, k, v, sinks):
    B, S = q.shape[:2]
    nb = S // SWA_BLOCK
    qb = q.reshape(B, nb, SWA_BLOCK, SWA_KV_HEADS, SWA_GROUP, HEAD_DIM)

    def band(t):
        tb = t.reshape(B, nb, SWA_BLOCK, SWA_KV_HEADS, HEAD_DIM)
        prev = jnp.concatenate([jnp.zeros_like(tb[:, :1]), tb[:, :-1]], axis=1)
        return jnp.concatenate([prev, tb], axis=2)

    kk, vv = band(k), band(v)
    s = jnp.einsum('bnqhgd,bnkhd->bnhgqk', qb, kk).astype(jnp.float32) / math.sqrt(HEAD_DIM)
    qi = jnp.arange(SWA_BLOCK)[:, None]
    kj = jnp.arange(2 * SWA_BLOCK)[None, :]
    rel = qi + SWA_BLOCK - kj
    local = (rel >= 0) & (rel < WINDOW)
    kpos = jnp.arange(nb)[:, None] * SWA_BLOCK - SWA_BLOCK + kj
    mask = local[None, None, None] & (kpos >= 0)[:, None, None, None, :]
    s = jnp.where(mask, s, NEG_INF)
    sink = jnp.broadcast_to(sinks.astype(jnp.float32).reshape(1, 1, SWA_KV_HEADS, SWA_GROUP, 1, 1),
                            s.shape[:-1] + (1,))
    p = jax.nn.softmax(jnp.concatenate([s, sink], axis=-1), axis=-1)[..., :-1]
    o = jnp.einsum('bnhgqk,bnkhd->bnqhgd', p.astype(v.dtype), vv)
    return o.reshape(B, S, SWA_Q_HEADS * HEAD_DIM)


def diff_attention(q, k, v, lam, lambda_init, subln_g):
    B, S = q.shape[:2]
    nb = S // Q_BLOCK
    qb = jnp.moveaxis(q.reshape(B, nb, Q_BLOCK, DIFF_HEADS, 2, HEAD_DIM), 1, 0)
    kpos = jnp.arange(S)

    def block(args):
        qblk, n = args
        s = jnp.einsum('bqhcd,bkhcd->bhcqk', qblk, k).astype(jnp.float32) / math.sqrt(HEAD_DIM)
        qpos = n * Q_BLOCK + jnp.arange(Q_BLOCK)
        s = jnp.where(kpos[None, :] <= qpos[:, None], s, NEG_INF)
        p = jax.nn.softmax(s, axis=-1)
        a = p[:, :, 0] - lam * p[:, :, 1]
        return jnp.einsum('bhqk,bkhe->bqhe', a.astype(v.dtype), v)

    o = lax.map(block, (qb, jnp.arange(nb)))
    o = jnp.moveaxis(o, 0, 1).reshape(B, S, DIFF_HEADS, DIFF_V_DIM)
    o = rmsnorm(o, subln_g) * (1.0 - lambda_init)
    return o.reshape(B, S, DIFF_HEADS * DIFF_V_DIM)


def hier_moe(h, w_rg, b_rg, w_re, b_re, w_gu, w_dn):
    N = h.shape[0]
    glog = (h @ w_rg).astype(jnp.float32) + b_rg.astype(jnp.float32)
    gprob = jax.nn.softmax(glog, axis=-1)
    gsel = jnp.argmax(glog, axis=-1)
    gweight = jnp.take_along_axis(gprob, gsel[:, None], axis=1)[:, 0]
    elog = ((h @ w_re).astype(jnp.float32) + b_re.astype(jnp.float32)).reshape(N, N_GROUPS, EXPERTS_PER_GROUP)
    elog_sel = jnp.take_along_axis(elog, gsel[:, None, None], axis=1)[:, 0]
    top_v, top_i = lax.top_k(elog_sel, TOP_K)
    weights = gweight[:, None] * jax.nn.softmax(top_v, axis=-1)
    expert_ids = (gsel[:, None] * EXPERTS_PER_GROUP + top_i).astype(jnp.int32)

    A = N * TOP_K
    flat_e = expert_ids.reshape(-1)
    flat_tok = jnp.repeat(jnp.arange(N, dtype=jnp.int32), TOP_K)
    flat_w = weights.reshape(-1)
    order = jnp.argsort(flat_e)
    sorted_e = flat_e[order]
    counts = jnp.bincount(flat_e, length=N_EXPERTS)
    padded = ((counts + MOE_BLOCK - 1) // MOE_BLOCK) * MOE_BLOCK
    pend = jnp.cumsum(padded)
    pstart = pend - padded
    start = jnp.cumsum(counts) - counts
    dest = pstart[sorted_e] + jnp.arange(A) - start[sorted_e]
    P = A + N_EXPERTS * MOE_BLOCK
    n_blocks = P // MOE_BLOCK
    row_tok = jnp.full((P,), N, dtype=jnp.int32).at[dest].set(flat_tok[order])
    row_w = jnp.zeros((P,), jnp.float32).at[dest].set(flat_w[order])
    block_e = jnp.minimum(jnp.searchsorted(pend, jnp.arange(n_blocks) * MOE_BLOCK, side='right'),
                          N_EXPERTS - 1).astype(jnp.int32)
    h_pad = jnp.concatenate([h, jnp.zeros((1, h.shape[1]), h.dtype)], axis=0)
    xs = h_pad[row_tok].reshape(n_blocks, MOE_BLOCK, h.shape[1])

    def run_block(args):
        xb, e = args
        g, u = jnp.split(xb @ w_gu[e], 2, axis=-1)
        return (jax.nn.silu(g) * u) @ w_dn[e]

    yb = lax.map(run_block, (xs, block_e)).reshape(P, h.shape[1])
    yb = (yb.astype(jnp.float32) * row_w[:, None]).astype(h.dtype)
    return jnp.zeros((N + 1, h.shape[1]), h.dtype).at[row_tok].add(yb)[:N]


def setup_inputs(seed: int = 0) -> dict:
    key = jax.random.key(seed)
    ks = jax.random.split(key, 24)
    D, L = D_MODEL, DEPTH
    nrm = lambda k, shp, s: jax.random.normal(k, shp, jnp.float32) * s
    return {
        "x": nrm(ks[0], (BATCH, SEQ, D), 1.0),
        "c": nrm(ks[1], (BATCH, D), 1.0),
        "positions": jnp.broadcast_to(jnp.arange(SEQ, dtype=jnp.int32)[None, :], (BATCH, SEQ)),
        "w_ada": nrm(ks[2], (L, D, 6 * D), 0.5 * D ** -0.5),
        "b_ada": nrm(ks[3], (L, 6 * D), 0.02),
        "g_mix": 1.0 + nrm(ks[4], (L, D), 0.02),
        "w_in": nrm(ks[5], (L, D, IN_COLS), D ** -0.5),
        "swa_sinks": nrm(ks[6], (L, SWA_Q_HEADS), 0.5),
        "diff_lambda_q1": nrm(ks[7], (L, HEAD_DIM), 0.1),
        "diff_lambda_k1": nrm(ks[8], (L, HEAD_DIM), 0.1),
        "diff_lambda_q2": nrm(ks[9], (L, HEAD_DIM), 0.1),
        "diff_lambda_k2": nrm(ks[10], (L, HEAD_DIM), 0.1),
        "diff_subln_g": 1.0 + nrm(ks[11], (L, DIFF_V_DIM), 0.02),
        "w_branch_a": nrm(ks[12], (L, SWA_Q_COLS, D), SWA_Q_COLS ** -0.5),
        "w_branch_b": nrm(ks[13], (L, DIFF_V_COLS, D), DIFF_V_COLS ** -0.5),
        "w_out": nrm(ks[14], (L, D, D), D ** -0.5),
        "g_ffn": 1.0 + nrm(ks[15], (L, D), 0.02),
        "w_router_group": nrm(ks[16], (L, D, N_GROUPS), D ** -0.5),
        "b_router_group": nrm(ks[17], (L, N_GROUPS), 0.01),
        "w_router_expert": nrm(ks[18], (L, D, N_EXPERTS), D ** -0.5),
        "b_router_expert": nrm(ks[19], (L, N_EXPERTS), 0.01),
        "w_exp_gate_up": nrm(ks[20], (L, N_EXPERTS, D, 2 * D_EXPERT), D ** -0.5),
        "w_exp_down": nrm(ks[21], (L, N_EXPERTS, D_EXPERT, D), D_EXPERT ** -0.5),
        "g_final": 1.0 + nrm(ks[22], (D,), 0.02),
    }


def reference(x, c, positions, w_ada, b_ada, g_mix, w_in, swa_sinks, diff_lambda_q1, diff_lambda_k1,
              diff_lambda_q2, diff_lambda_k2, diff_subln_g, w_branch_a, w_branch_b, w_out, g_ffn,
              w_router_group, b_router_group, w_router_expert, b_router_expert, w_exp_gate_up,
              w_exp_down, g_final):
    B, S, D = x.shape
    cos, sin = rope_tables(positions)
    c_act = jax.nn.silu(c)
    splits = np.cumsum([SWA_Q_COLS, SWA_KV_COLS, SWA_KV_COLS, DIFF_QK_COLS, DIFF_QK_COLS, DIFF_V_COLS, D_MODEL])
    for layer in range(DEPTH):
        lambda_init = 0.8 - 0.6 * math.exp(-0.3 * layer)
        mod = c_act @ w_ada[layer] + b_ada[layer]
        sh_m, sc_m, gt_m, sh_f, sc_f, gt_f = [m[:, None, :] for m in jnp.split(mod, 6, axis=-1)]

        h = rmsnorm(x, g_mix[layer]) * (1.0 + sc_m) + sh_m
        proj = h @ w_in[layer]
        qa, ka, va, qd, kd, vd, ga, gb = jnp.split(proj, splits, axis=-1)
        qa = apply_rope(qa.reshape(B, S, SWA_Q_HEADS, HEAD_DIM), cos, sin)
        ka = apply_rope(ka.reshape(B, S, SWA_KV_HEADS, HEAD_DIM), cos, sin)
        va = va.reshape(B, S, SWA_KV_HEADS, HEAD_DIM)
        qd = apply_rope(qd.reshape(B, S, DIFF_HEADS, 2, HEAD_DIM), cos, sin)
        kd = apply_rope(kd.reshape(B, S, DIFF_HEADS, 2, HEAD_DIM), cos, sin)
        vd = vd.reshape(B, S, DIFF_HEADS, DIFF_V_DIM)
        lam = (jnp.exp(jnp.sum(diff_lambda_q1[layer].astype(jnp.float32) * diff_lambda_k1[layer].astype(jnp.float32)))
               - jnp.exp(jnp.sum(diff_lambda_q2[layer].astype(jnp.float32) * diff_lambda_k2[layer].astype(jnp.float32)))
               + lambda_init)
        o_a = swa_sink_attention(qa, ka, va, swa_sinks[layer])
        o_b = diff_attention(qd, kd, vd, lam, lambda_init, diff_subln_g[layer])
        merged = jax.nn.sigmoid(ga) * (o_a @ w_branch_a[layer]) + jax.nn.sigmoid(gb) * (o_b @ w_branch_b[layer])
        x = x + gt_m * (merged @ w_out[layer])

        h2 = rmsnorm(x, g_ffn[layer]) * (1.0 + sc_f) + sh_f
        ffn = hier_moe(h2.reshape(B * S, D), w_router_group[layer], b_router_group[layer],
                       w_router_expert[layer], b_router_expert[layer],
                       w_exp_gate_up[layer], w_exp_down[layer]).reshape(B, S, D)
        x = x + gt_f * ffn
    return rmsnorm(x, g_final)
```

```python
import math
import types
from contextlib import ExitStack
import numpy as np
import ml_dtypes
import concourse.bass as bass
import concourse.mybir as mybir
from concourse.bass_utils import run_bass_kernel_spmd

F32, BF16, I32 = mybir.dt.float32, mybir.dt.bfloat16, mybir.dt.int32
ALU = mybir.AluOpType
AF = mybir.ActivationFunctionType
AX = mybir.AxisListType
DTB = {F32: 4, BF16: 2, I32: 4}

NCORES = 8
T = 2048
D = 2048
NB = T // 128
CAP = 384
NSB = CAP // 128
NE = 64
EPS = 1e-6
NDS = 8
LAMBDA_INIT = 0.8 - 0.6 * math.exp(-0.3 * 0)


def _freeze(fn):
    if fn.__closure__ is None:
        return fn
    cells = []
    for c in fn.__closure__:
        try:
            cells.append(types.CellType(c.cell_contents))
        except ValueError:
            cells.append(c)
    return types.FunctionType(fn.__code__, fn.__globals__, fn.__name__, fn.__defaults__, tuple(cells))


class Prog:
    ENGS = ("pe", "act", "dve", "pool", "sp")

    def __init__(self, nc, es):
        self.nc = nc
        self.ops = []
        self.lastw = {}
        self.readers = {}
        self.esem = {e: es.enter_context(nc.semaphore("s_" + e)) for e in self.ENGS}
        self.dsem = {q: [es.enter_context(nc.semaphore(f"d_{q}_{i}")) for i in range(NDS)]
                     for q in ("sp", "act", "pool")}

    def add(self, eng, fn, reads=(), writes=(), dma=False):
        i = len(self.ops)
        deps = set()
        reads = list(reads) + ["__phase"]
        for k in reads:
            if k in self.lastw:
                deps.add(self.lastw[k])
            if k[:2] in ("pA", "pB", "pC"):
                for r in self.readers.get(k, ()):
                    if self.ops[r]["eng"] != eng:
                        deps.add(r)
        for k in writes:
            if k in self.lastw:
                deps.add(self.lastw[k])
            for r in self.readers.get(k, ()):
                deps.add(r)
        self.ops.append(dict(eng=eng, fn=_freeze(fn), deps=deps, dma=dma))
        for k in reads:
            self.readers.setdefault(k, []).append(i)
        for k in writes:
            self.lastw[k] = i
            self.readers[k] = []
        return i

    def dma(self, q, out, in_, reads=(), writes=(), **kw):
        return self.add(q, lambda e: e.dma_start(out=out, in_=in_, **kw), reads, writes, dma=True)

    def barrier(self):
        i = len(self.ops)
        deps = set(self.readers.get("__phase", ()))
        if "__phase" in self.lastw:
            deps.add(self.lastw["__phase"])
        self.ops.append(dict(eng="sp", fn=lambda e: e.nop(), deps=deps, dma=False))
        self.lastw["__phase"] = i
        self.readers["__phase"] = []

    def emit(self):
        ops = self.ops
        qcount = {q: 0 for q in self.dsem}
        qhist = {q: [] for q in self.dsem}
        for i, op in enumerate(ops):
            if op["dma"]:
                q = op["eng"]
                k = qcount[q]
                qcount[q] += 1
                op["sem"] = self.dsem[q][k % NDS]
                op["val"] = 16 * (k // NDS + 1)
                if k >= NDS:
                    op["deps"].add(qhist[q][k - NDS])
                qhist[q].append(i)

        def skip(od, op):
            return od["eng"] == "pe" and op["eng"] == "pe" and not od["dma"] and not op["dma"]

        needed = [False] * len(ops)
        for i, op in enumerate(ops):
            for d in op["deps"]:
                if not skip(ops[d], op):
                    needed[d] = True
        cnt = {e: 0 for e in self.ENGS}
        for i, op in enumerate(ops):
            if op["dma"]:
                continue
            if needed[i]:
                cnt[op["eng"]] += 1
                op["sem"] = self.esem[op["eng"]]
                op["val"] = cnt[op["eng"]]
        seen = {e: {} for e in self.ENGS}
        per = {e: [] for e in self.ENGS}
        for i, op in enumerate(ops):
            e = op["eng"]
            waits = {}
            for d in sorted(op["deps"]):
                od = ops[d]
                if skip(od, op):
                    continue
                s, v = od["sem"], od["val"]
                key = id(s)
                if seen[e].get(key, 0) >= v:
                    continue
                seen[e][key] = v
                waits[key] = (s, v)
            op["waits"] = list(waits.values())
            op["signal"] = op["dma"] or needed[i]
            per[e].append(op)

        def mk(name):
            def body(e):
                for op in per[name]:
                    for s, v in op["waits"]:
                        e.wait_ge(s, v)
                    ins = op["fn"](e)
                    if op["signal"]:
                        ins.then_inc(op["sem"], 16 if op["dma"] else 1)
            return body

        with self.nc.Block() as block:
            block.tensor(mk("pe"))
            block.scalar(mk("act"))
            block.vector(mk("dve"))
            block.gpsimd(mk("pool"))
            block.sync(mk("sp"))


class Arena:
    def __init__(self, nc, es, name, nbytes):
        self.t = es.enter_context(nc.sbuf_tensor(name, [128, nbytes // 2], BF16))
        self.cap = nbytes
        self.top = 0

    def alloc(self, shape, dt):
        n = 1
        for s in shape[1:]:
            n *= s
        nb = (n * DTB[dt] + 31) // 32 * 32
        assert self.top + nb <= self.cap, (self.top, nb, self.cap, shape)
        v = self.t[0:shape[0], self.top // 2:(self.top + n * DTB[dt]) // 2]
        self.top += nb
        if dt != BF16:
            v = v.bitcast(dt)
        if len(shape) == 3:
            v = v.rearrange("p (a b) -> p a b", b=shape[2])
        elif len(shape) == 4:
            v = v.rearrange("p (a b c) -> p a b c", b=shape[2], c=shape[3])
        return v

    def reset(self, mark=0):
        self.top = mark


def build_program(debug=False, stop=None):
    try:
        return _build_program(debug, stop)
    except StopIteration as e:
        return e.value


def _build_program(debug=False, stop=None):
    nc = bass.Bass("TRN2", target_bir_lowering=False)
    es = ExitStack()

    def din(name, shape, dt=F32):
        return nc.dram_tensor(name, list(shape), dt, kind="ExternalInput")

    x_d = din("x", [T, D])
    cT_d = din("cT", [128, 16])
    pos_d = din("pos", [128, NB], I32)
    wada_d = din("w_ada", [D, 6 * D])
    bada_d = din("b_ada", [1, 6 * D])
    gmixT_d = din("g_mixT", [128, 16])
    win_d = din("w_in", [D, 8448])
    sinks_d = din("sinks", [128, 16])
    lam_d = din("lamv", [128, 4, 64])
    subln_d = din("subln", [128, 128])
    wa_d = din("w_a", [1024, D])
    wb_d = din("w_b", [1024, D])
    wo_d = din("w_o", [D, D])
    gffn_d = din("g_ffn", [128, D])
    wr_d = din("w_r", [D, 72])
    br_d = din("b_r", [128, 72])
    if stop is None or stop == "full":
        wgu_d = din("w_gu", [NE, D, D])
        wdn_d = din("w_dn", [NE, 1024, D])
    gfin_d = din("g_fin", [128, D])
    identb_d = din("ident_b", [128, 128], BF16)
    identf_d = din("ident_f", [128, 128])
    utri_d = din("utri", [128, 128])
    causal_d = din("causal", [128, 128], BF16)
    swam_d = din("swamask", [128, 256], BF16)
    invf_d = din("invfreq", [128, 32])
    eidx_d = din("eidx", [128, NE])
    out_d = nc.dram_tensor("out", [T, D], F32, kind="ExternalOutput")
    if debug:
        dbg_x1 = nc.dram_tensor("dbg_x1", [T, D], F32, kind="ExternalOutput")
        dbg_o = nc.dram_tensor("dbg_o", [16, 128, T], BF16, kind="ExternalOutput")
        dbg_dest = nc.dram_tensor("dbg_dest", [128, NB, 2, 4], F32, kind="ExternalOutput")
        dbg_w = nc.dram_tensor("dbg_w", [128, NB, 2], F32, kind="ExternalOutput")
        dbg_mod = nc.dram_tensor("dbg_mod", [96, 128], F32, kind="ExternalOutput")

    mod_d = nc.dram_tensor("mod_s", [6 * D], F32)
    qT_d = nc.dram_tensor("qT_s", [25, 128, T], BF16)
    v_d = nc.dram_tensor("v_s", [T, 1152], BF16)
    sg_d = nc.dram_tensor("sg_s", [T, 4096], BF16)
    x1_d = nc.dram_tensor("x1_s", [T, D], F32)
    xs_d = nc.dram_tensor("xs_s", [NE * CAP + 128, D], BF16)
    ys_d = nc.dram_tensor("ys_s", [NE * CAP + 128, D], BF16)

    P = Prog(nc, es)
    R0 = Arena(nc, es, "R0", 64 * 1024)
    R1 = Arena(nc, es, "R1", 64 * 1024)
    RP = Arena(nc, es, "RP", 21 * 1024)
    RF = Arena(nc, es, "RF", 52 * 1024)

    def ps(name, shape, dt=F32):
        return es.enter_context(nc.psum_tensor(name, list(shape), dt))


    def maybe_stop(tag):
        if stop == tag:
            P.barrier()
            P.dma("sp", out_d[0:128, :], x_d[0:128, :], writes=["out_d"])
            P.barrier()
            P.emit()
            es.close()
            raise StopIteration(nc)

    pA = [ps(f"pA{i}", [128, 512])[:] for i in range(4)]
    pB = [ps(f"pB{i}", [128, 8, 128], BF16)[:] for i in range(2)]
    pC = [ps(f"pC{i}", [128, 512])[:] for i in range(2)]

    ctr = {}

    def rot(lst, name):
        i = ctr.get(name, 0)
        ctr[name] = i + 1
        return i % len(lst), lst[i % len(lst)]

    ident_b = RP.alloc([128, 128], BF16)
    ident_f = RP.alloc([128, 128], F32)
    utri_s = RP.alloc([128, 128], F32)
    ones_s = RP.alloc([128, 128], F32)
    causal_s = RP.alloc([128, 128], BF16)
    swam_s = RP.alloc([128, 256], BF16)
    modT = RP.alloc([128, 96], F32)
    a1T = RP.alloc([128, 16], F32)
    esink = RP.alloc([128, 16], F32)
    lam_s = RP.alloc([128, 8], F32)
    subln_s = RP.alloc([128, 128], F32)
    stats = RP.alloc([128, 16], F32)
    small = RP.alloc([128, 16], F32)
    bcX = RP.alloc([128, D], F32)
    bcY = RP.alloc([128, D], F32)
    wsel2 = RP.alloc([128, NB, 2], F32)
    dest_i = RP.alloc([128, NB, 2], I32)
    RP_mark = RP.top

    P.dma("sp", ident_b, identb_d[:, :], writes=["ident_b"])
    P.dma("sp", ident_f, identf_d[:, :], writes=["ident_f"])
    P.dma("sp", utri_s, utri_d[:, :], writes=["utri"])
    P.dma("sp", causal_s, causal_d[:, :], writes=["causal"])
    P.dma("sp", swam_s, swam_d[:, :], writes=["swam"])
    P.add("pool", lambda e: e.memset(ones_s, 1.0), writes=["ones"])
    P.dma("sp", subln_s, subln_d[:, :], writes=["subln"])
    P.dma("sp", esink, sinks_d[:, :], writes=["esink"])
    P.add("act", lambda e: e.activation(out=esink, in_=esink, func=AF.Exp), reads=["esink"], writes=["esink"])

    maybe_stop("c0")

    wring = [RF.alloc([128, 16, 512], BF16) for _ in range(2)]
    RF_mark = RF.top

    def load_w(src2d, kc, c0, width):
        i, wt = rot(wring, "wring")
        key = f"wring{i}"
        P.dma("pool", wt[:, 0:kc, 0:width], src2d[:, c0:c0 + width].rearrange("(k p) n -> p k n", p=128), writes=[key, key + "b"])
        return wt, key

    def pipelined(pieces, loader, compute):
        nxt = loader(pieces[0])
        for i, pc in enumerate(pieces):
            cur = nxt
            if i + 1 < len(pieces):
                nxt = loader(pieces[i + 1])
            compute(pc, cur)

    cos_s = R1.alloc([128, NB, 32], F32)
    sin_s = R1.alloc([128, NB, 32], F32)
    pos_i = R1.alloc([128, NB], I32)
    pos_f = R1.alloc([128, NB], F32)
    invf = R1.alloc([128, 32], F32)
    lamv = R1.alloc([128, 4, 64], F32)
    cT_s = R1.alloc([128, 16], F32)
    cact = R1.alloc([128, 16], BF16)
    gmixT = R1.alloc([128, 16], F32)
    brow = [R1.alloc([1, 512], F32) for _ in range(2)]
    mrow = [R1.alloc([1, 512], F32) for _ in range(2)]
    modR = R1.alloc([96, 128], F32)
    rr_i = R1.alloc([128, NB, 32], I32)
    rr_f = R1.alloc([128, NB, 32], F32)
    R1_p10 = R1.top

    P.dma("sp", pos_i, pos_d[:, :], writes=["pos_i"])
    P.dma("sp", invf, invf_d[:, :], writes=["invf"])
    P.add("dve", lambda e: e.tensor_copy(pos_f, pos_i), reads=["pos_i"], writes=["pos_f"])
    for blk in range(NB):
        P.add("dve", lambda e, blk=blk: e.tensor_scalar(out=sin_s[:, blk, :], in0=invf, scalar1=pos_f[:, blk:blk + 1], scalar2=None, op0=ALU.mult),
              reads=["pos_f", "invf"], writes=["sin"])
    P.add("dve", lambda e: e.tensor_scalar(out=cos_s, in0=sin_s, scalar1=0.25, scalar2=None, op0=ALU.add), reads=["sin"], writes=["cos"])
    for tab, nm in ((sin_s, "sin"), (cos_s, "cos")):
        P.add("dve", lambda e, tab=tab: e.tensor_copy(rr_i, tab), reads=[nm], writes=["rr_i"])
        P.add("dve", lambda e, tab=tab: e.tensor_copy(rr_f, rr_i), reads=["rr_i"], writes=["rr_f"])
        P.add("dve", lambda e, tab=tab: e.tensor_tensor(out=tab, in0=tab, in1=rr_f, op=ALU.subtract), reads=[nm, "rr_f"], writes=[nm])
        P.add("act", lambda e, tab=tab: e.activation(out=tab, in_=tab, func=AF.Sin, scale=2.0 * math.pi), reads=[nm], writes=[nm])

    maybe_stop("c1")
    P.dma("sp", lamv, lam_d[:, :, :], writes=["lamv"])
    P.add("dve", lambda e: e.tensor_tensor(out=lamv[:, 0, :], in0=lamv[:, 0, :], in1=lamv[:, 1, :], op=ALU.mult), reads=["lamv"], writes=["lamv"])
    P.add("dve", lambda e: e.tensor_tensor(out=lamv[:, 2, :], in0=lamv[:, 2, :], in1=lamv[:, 3, :], op=ALU.mult), reads=["lamv"], writes=["lamv"])
    P.add("dve", lambda e: e.reduce_sum(out=lam_s[:, 0:1], in_=lamv[:, 0, :], axis=AX.X), reads=["lamv"], writes=["lam"])
    P.add("dve", lambda e: e.reduce_sum(out=lam_s[:, 1:2], in_=lamv[:, 2, :], axis=AX.X), reads=["lamv"], writes=["lam"])
    P.add("act", lambda e: e.activation(out=lam_s[:, 0:2], in_=lam_s[:, 0:2], func=AF.Exp), reads=["lam"], writes=["lam"])
    P.add("dve", lambda e: e.tensor_tensor(out=lam_s[:, 2:3], in0=lam_s[:, 1:2], in1=lam_s[:, 0:1], op=ALU.subtract), reads=["lam"], writes=["lam"])
    P.add("dve", lambda e: e.tensor_scalar(out=lam_s[:, 2:3], in0=lam_s[:, 2:3], scalar1=-LAMBDA_INIT, scalar2=None, op0=ALU.add),
          reads=["lam"], writes=["lam"])
    P.add("dve", lambda e: e.tensor_scalar(out=subln_s, in0=subln_s, scalar1=1.0 - LAMBDA_INIT, scalar2=None, op0=ALU.mult),
          reads=["subln"], writes=["subln"])

    maybe_stop("c2")
    P.dma("sp", cT_s, cT_d[:, :], writes=["cT"])
    P.add("act", lambda e: e.activation(out=cact, in_=cT_s, func=AF.Silu), reads=["cT"], writes=["cact"])
    mod2 = mod_d.ap().rearrange("(a n) -> a n", a=1)

    def ada_compute(g, cur):
        wt, wk = cur
        pi, pt = rot(pA, "pA")
        bi, br = rot(brow, "brow")
        P.dma("sp", br, bada_d[:, g * 512:(g + 1) * 512], writes=[f"brow{bi}"])
        for k in range(16):
            P.add("pe", lambda e, pt=pt, wt=wt, k=k: e.matmul(pt[0:1, :], lhsT=cact[:, k:k + 1], rhs=wt[:, k, :], start=(k == 0), stop=(k == 15)),
                  reads=["cact", wk], writes=[f"pA{pi}"])
        mi, mr = rot(mrow, "mrow")
        P.add("dve", lambda e, pt=pt, mr=mr, br=br: e.tensor_tensor(out=mr, in0=pt[0:1, :], in1=br, op=ALU.add),
              reads=[f"pA{pi}", f"brow{bi}"], writes=[f"mrow{mi}"])
        P.dma("sp", mod2[:, g * 512:(g + 1) * 512], mr, reads=[f"mrow{mi}"], writes=["mod_d"])

    pipelined(list(range(24)), lambda g: load_w(wada_d, 16, g * 512, 512), ada_compute)
    maybe_stop("c3")
    P.dma("sp", modR, mod_d.ap().rearrange("(c p) -> c p", p=128), reads=["mod_d"], writes=["modR"])
    P.add("pe", lambda e: e.transpose(pC[0][:, 0:96], modR, ident_f[0:96, 0:96]), reads=["modR", "ident_f"], writes=["pC0"])
    P.add("dve", lambda e: e.tensor_copy(modT, pC[0][:, 0:96]), reads=["pC0"], writes=["modT"])
    maybe_stop("c4")
    P.dma("sp", gmixT, gmixT_d[:, :], writes=["gmixT"])
    P.add("dve", lambda e: e.scalar_tensor_tensor(out=a1T, in0=modT[:, 16:32], scalar=1.0, in1=gmixT, op0=ALU.add, op1=ALU.mult),
          reads=["modT", "gmixT"], writes=["a1T"])
    maybe_stop("c5")
    if debug:
        P.dma("sp", dbg_mod[:, :], modR, reads=["modR"], writes=["dbg_mod"])

    maybe_stop("p10")

    def rms_rstd(src, key_src, col, n, junk, junk_key):
        P.add("act", lambda e: e.activation(out=junk, in_=src, func=AF.Square, accum_out=stats[:, col:col + 1]),
              reads=[key_src], writes=[junk_key, f"stats{col}"])
        P.add("act", lambda e: e.activation(out=stats[:, col:col + 1], in_=stats[:, col:col + 1], func=AF.Sqrt, scale=1.0 / n, bias=EPS),
              reads=[f"stats{col}"], writes=[f"stats{col}"])
        P.add("dve", lambda e: e.reciprocal(stats[:, col:col + 1], stats[:, col:col + 1]), reads=[f"stats{col}"], writes=[f"stats{col}"])

    def transpose_blocks(src_fn, src_key, nchunks, dst_fn, dst_key_fn, evac=None, rows=128):
        for c0 in range(0, nchunks, 4):
            n = min(4, nchunks - c0)
            bi, pb = rot(pB, "pB")
            for j in range(n):
                src_ap = src_fn(c0 + j)
                P.add("pe", lambda e, pb=pb, j=j, src_ap=src_ap: e.transpose(pb[0:rows, j, :], src_ap, ident_b),
                      reads=[src_key, "ident_b"], writes=[f"pB{bi}"])
            for j in range(n):
                c = c0 + j
                if evac is None:
                    dst_ap = dst_fn(c)
                    P.add("dve", lambda e, pb=pb, j=j, dst_ap=dst_ap: e.tensor_copy(dst_ap, pb[0:rows, j, :]), reads=[f"pB{bi}"],
                          writes=[dst_key_fn(c)])
                else:
                    evac(pb[0:rows, j, :], f"pB{bi}", c)

    hT = R0.alloc([128, 16, T], BF16)
    xt = [R1.alloc([128, D], F32) for _ in range(2)]
    xn = R1.alloc([128, D], BF16)
    sq_junk = R1.alloc([128, D], BF16)
    for blk in range(NB):
        xi, xtile = rot(xt, "xt")
        P.dma("sp", xtile, x_d[blk * 128:(blk + 1) * 128, :], writes=[f"xt{xi}"])
        rms_rstd(xtile, f"xt{xi}", 0, D, sq_junk, "sq_junk")
        P.add("dve", lambda e, xtile=xtile: e.tensor_scalar(out=xn, in0=xtile, scalar1=stats[:, 0:1], scalar2=None, op0=ALU.mult),
              reads=[f"xt{xi}", "stats0"], writes=["xn"])

        def evac_h(psrc, pkey, c, blk=blk):
            P.add("act", lambda e: e.activation(out=hT[:, c, blk * 128:(blk + 1) * 128], in_=psrc, func=AF.Identity,
                                                bias=modT[:, c:c + 1], scale=a1T[:, c:c + 1]),
                  reads=[pkey, "modT", "a1T"], writes=[f"hT{blk}"])
        transpose_blocks(lambda c: xn[:, c * 128:(c + 1) * 128], "xn", 16, None, None, evac=evac_h)
    maybe_stop("p11")
    P.barrier()
    RF.reset(RF_mark)

    stage = [RF.alloc([128, 512], BF16) for _ in range(4)]
    rt = [RF.alloc([128, 256], F32) for _ in range(4)]
    groups = [(0, 512, "q"), (512, 512, "q"), (1024, 256, "kv"), (1280, 512, "q"), (1792, 512, "q"),
              (2304, 512, "q"), (2816, 512, "q"), (3328, 512, "v"), (3840, 512, "v")] + \
             [(4352 + 512 * i, 512, "g") for i in range(8)]

    def qchunk_of(col):
        if col < 1024:
            return col // 128
        if col < 1152:
            return 8
        return 9 + (col - 1280) // 128

    def win_compute(grp, cur):
        c0, width, kind = grp
        wt, wk = cur
        for blk in range(NB):
            pi, pt = rot(pA, "pA")
            pk = f"pA{pi}"
            for k in range(16):
                P.add("pe", lambda e, pt=pt, wt=wt, k=k, blk=blk: e.matmul(pt[:, 0:width], lhsT=hT[:, k, blk * 128:(blk + 1) * 128],
                                                                           rhs=wt[:, k, 0:width], start=(k == 0), stop=(k == 15)),
                      reads=[f"hT{blk}", wk], writes=[pk])
            si, st = rot(stage, "stage")
            sk = f"stage{si}"
            ropew = width if kind == "q" else (128 if kind == "kv" else 0)
            if stop == "p12:kv2":
                ropew = 0
            if ropew:
                nh = ropew // 64
                pv = pt[:, 0:ropew].rearrange("p (h t i) -> p h t i", t=2, i=32)
                sv = st[:, 0:ropew].rearrange("p (h t i) -> p h t i", t=2, i=32)
                cosb = cos_s[:, blk, :].unsqueeze(1).to_broadcast([128, nh, 32])
                sinb = sin_s[:, blk, :].unsqueeze(1).to_broadcast([128, nh, 32])
                r = [rt[q][:, 0:ropew // 2].rearrange("p (h i) -> p h i", i=32) for q in range(4)]
                P.add("dve", lambda e: e.tensor_tensor(out=r[0], in0=pv[:, :, 0, :], in1=cosb, op=ALU.mult), reads=[pk, "cos"], writes=["rt0"])
                P.add("dve", lambda e: e.tensor_tensor(out=r[1], in0=pv[:, :, 1, :], in1=sinb, op=ALU.mult), reads=[pk, "sin"], writes=["rt1"])
                P.add("dve", lambda e: e.tensor_tensor(out=r[2], in0=pv[:, :, 1, :], in1=cosb, op=ALU.mult), reads=[pk, "cos"], writes=["rt2"])
                P.add("dve", lambda e: e.tensor_tensor(out=r[3], in0=pv[:, :, 0, :], in1=sinb, op=ALU.mult), reads=[pk, "sin"], writes=["rt3"])
                P.add("pool", lambda e: e.tensor_tensor(out=sv[:, :, 0, :], in0=r[0], in1=r[1], op=ALU.subtract), reads=["rt0", "rt1"], writes=[sk])
                P.add("pool", lambda e: e.tensor_tensor(out=sv[:, :, 1, :], in0=r[2], in1=r[3], op=ALU.add), reads=["rt2", "rt3"], writes=[sk])
                nch = ropew // 128
                s2i, st2 = rot(stage, "stage")
                s2k = f"stage{s2i}"
                transpose_blocks(lambda c: st[:, c * 128:(c + 1) * 128], sk, nch, lambda c: st2[:, c * 128:(c + 1) * 128], lambda c: s2k)
                qc0 = qchunk_of(c0)
                if nch == 1:
                    P.dma("sp", qT_d[qc0, :, blk * 128:(blk + 1) * 128], st2[:, 0:128], reads=[s2k], writes=["qT_d"])
                else:
                    P.dma("sp", qT_d[qc0:qc0 + nch, :, blk * 128:(blk + 1) * 128].rearrange("c p t -> p c t"),
                          st2[:, 0:nch * 128].rearrange("p (c t) -> p c t", t=128), reads=[s2k], writes=["qT_d"])
                if kind == "kv" and stop != "p12:kv1":
                    s3i, st3 = rot(stage, "stage")
                    P.add("act", lambda e: e.copy(st3[:, 0:128], pt[:, 128:256]), reads=[pk], writes=[f"stage{s3i}"])
                    P.dma("sp", v_d[blk * 128:(blk + 1) * 128, 0:128], st3[:, 0:128], reads=[f"stage{s3i}"], writes=["v_d"])
            elif kind == "kv":
                s3i, st3 = rot(stage, "stage")
                P.add("act", lambda e: e.copy(st3[:, 0:128], pt[:, 128:256]), reads=[pk], writes=[f"stage{s3i}"])
                P.dma("sp", v_d[blk * 128:(blk + 1) * 128, 0:128], st3[:, 0:128], reads=[f"stage{s3i}"], writes=["v_d"])
            elif kind == "v":
                P.add("act", lambda e: e.copy(st, pt), reads=[pk], writes=[sk])
                vc = 128 + (c0 - 3328)
                P.dma("sp", v_d[blk * 128:(blk + 1) * 128, vc:vc + 512], st, reads=[sk], writes=["v_d"])
            else:
                P.add("act", lambda e: e.activation(out=st, in_=pt, func=AF.Sigmoid), reads=[pk], writes=[sk])
                gc = c0 - 4352
                P.dma("sp", sg_d[blk * 128:(blk + 1) * 128, gc:gc + 512], st, reads=[sk], writes=["sg_d"])

    if stop is not None and stop.startswith("p12:"):
        groups = [g_ for g_ in groups if g_[2] == stop[4:6].rstrip("0123456789")][:1]
    pipelined(groups, lambda grp: load_w(win_d, 16, grp[0], grp[1]), win_compute)
    if stop is not None and stop.startswith("p12"):
        maybe_stop(stop)
    P.barrier()
    RF.reset(RF_mark)
    R0.reset()
    R1.reset()

    oT = R1.alloc([128, 16, T], BF16)
    qts = [R0.alloc([128, T], BF16) for _ in range(2)]
    kts = [R0.alloc([128, T], BF16) for _ in range(2)]
    vts = [R0.alloc([128, NB, 130], BF16) for _ in range(2)]
    pts = [R0.alloc([128, 512], BF16) for _ in range(4)]
    ob = [R0.alloc([128, 128], BF16) for _ in range(2)]
    ot = [R0.alloc([128, 128], F32) for _ in range(2)]
    ojunk = R0.alloc([128, 128], BF16)
    for i in range(2):
        P.add("pool", lambda e, i=i: e.memset(vts[i], 1.0), writes=[f"vts{i}"])

    for j in range(8):
        g = j // 4
        qi, qt = rot(qts, "qts")
        ki, kt = rot(kts, "kts")
        vi, vt = rot(vts, "vts")
        P.dma("sp", qt, qT_d[j, :, :], reads=["qT_d"], writes=[f"qts{qi}"])
        P.dma("sp", kt[0:64, :], qT_d[8, 64 * g:64 * g + 64, :], reads=["qT_d"], writes=[f"kts{ki}"])
        P.dma("sp", kt[64:128, :], qT_d[8, 64 * g:64 * g + 64, :], reads=["qT_d"], writes=[f"kts{ki}"])
        P.dma("sp", vt[:, :, 0:64], v_d[:, 64 * g:64 * g + 64].rearrange("(b p) e -> p b e", p=128), reads=["v_d"], writes=[f"vts{vi}"])
        for kc in range(NB):
            nq = 256 if kc < NB - 1 else 128
            oi, obt = rot(ob, "ob")
            for hh in range(2):
                h = 2 * j + hh
                lo = 64 * hh
                pi, pt = rot(pA, "pA")
                P.add("pe", lambda e, pt=pt, kt=kt, qt=qt, kc=kc, nq=nq, lo=lo: e.matmul(
                    pt[:, 0:nq], lhsT=kt[lo:lo + 64, kc * 128:(kc + 1) * 128], rhs=qt[lo:lo + 64, kc * 128:kc * 128 + nq],
                    start=True, stop=True), reads=[f"kts{ki}", f"qts{qi}"], writes=[f"pA{pi}"])
                ppi = 2 * hh + (kc % 2)
                pp = pts[ppi]
                P.add("act", lambda e, pp=pp, pt=pt, nq=nq: e.activation(out=pp[:, 0:nq], in_=pt[:, 0:nq], func=AF.Exp, scale=0.125),
                      reads=[f"pA{pi}"], writes=[f"pts{ppi}"])
                P.add("pool", lambda e, pp=pp, nq=nq: e.tensor_tensor(out=pp[:, 0:nq], in0=pp[:, 0:nq], in1=swam_s[:, 0:nq], op=ALU.mult),
                      reads=[f"pts{ppi}", "swam"], writes=[f"pts{ppi}"])
                ci, pc = rot(pC, "pC")
                if kc > 0:
                    pppi = 2 * hh + ((kc - 1) % 2)
                    ppp = pts[pppi]
                    P.add("pe", lambda e, pc=pc, ppp=ppp, vt=vt, kc=kc, lo=lo: e.matmul(pc[:, 0:65], lhsT=ppp[:, 128:256], rhs=vt[:, kc - 1, 0:65],
                                                                                      start=True, stop=False),
                          reads=[f"pts{pppi}", f"vts{vi}"], writes=[f"pC{ci}"])
                P.add("pe", lambda e, pc=pc, pp=pp, vt=vt, kc=kc: e.matmul(pc[:, 0:65], lhsT=pp[:, 0:128], rhs=vt[:, kc, 0:65],
                                                                           start=(kc == 0), stop=True),
                      reads=[f"pts{ppi}", f"vts{vi}"], writes=[f"pC{ci}"])
                P.add("dve", lambda e, pc=pc, h=h: e.tensor_scalar(out=small[:, 0:1], in0=pc[:, 64:65], scalar1=esink[:, h:h + 1], scalar2=None,
                                                                   op0=ALU.add), reads=[f"pC{ci}", "esink"], writes=["small0"])
                P.add("dve", lambda e: e.reciprocal(small[:, 1:2], small[:, 0:1]), reads=["small0"], writes=["small1"])
                P.add("dve", lambda e, pc=pc, obt=obt, lo=lo: e.tensor_scalar(out=obt[:, lo:lo + 64], in0=pc[:, 0:64], scalar1=small[:, 1:2],
                                                                              scalar2=None, op0=ALU.mult),
                      reads=[f"pC{ci}", "small1"], writes=[f"ob{oi}"])
            transpose_blocks(lambda c, obt=obt: obt, f"ob{oi}", 1, lambda c, j=j, kc=kc: oT[:, j, kc * 128:(kc + 1) * 128],
                             lambda c, kc=kc: f"oT{kc}")

    maybe_stop("p13a")
    for h in range(8):
        qi, qt = rot(qts, "qts")
        ki, kt = rot(kts, "kts")
        vi, vt = rot(vts, "vts")
        P.dma("sp", qt, qT_d[9 + h, :, :], reads=["qT_d"], writes=[f"qts{qi}"])
        P.dma("sp", kt, qT_d[17 + h, :, :], reads=["qT_d"], writes=[f"kts{ki}"])
        P.dma("sp", vt[:, :, 0:128], v_d[:, 128 + 128 * h:256 + 128 * h].rearrange("(b p) e -> p b e", p=128), reads=["v_d"], writes=[f"vts{vi}"])
        accs = [pA[2], pA[3], pC[0], pC[1]]
        acck = ["pA2", "pA3", "pC0", "pC1"]
        for Q in range(4):
            nkc = 4 * Q + 4
            for c in range(2):
                lo = 64 * c
                for kc in range(nkc):
                    j = kc - 4 * Q
                    q0 = 128 * j if j > 0 else 0
                    pi = ctr.get("pA01", 0) % 2
                    ctr["pA01"] = ctr.get("pA01", 0) + 1
                    pt = pA[pi]
                    P.add("pe", lambda e, pt=pt, kt=kt, qt=qt, kc=kc, lo=lo, q0=q0, Q=Q: e.matmul(
                        pt[:, q0:512], lhsT=kt[lo:lo + 64, kc * 128:(kc + 1) * 128], rhs=qt[lo:lo + 64, Q * 512 + q0:(Q + 1) * 512],
                        start=True, stop=True), reads=[f"kts{ki}", f"qts{qi}"], writes=[f"pA{pi}"])
                    ppi, pp = rot(pts, "pts")
                    P.add("act", lambda e, pp=pp, pt=pt, q0=q0: e.activation(out=pp[:, q0:512], in_=pt[:, q0:512], func=AF.Exp, scale=0.125),
                          reads=[f"pA{pi}"], writes=[f"pts{ppi}"])
                    if j >= 0:
                        P.add("pool", lambda e, pp=pp, q0=q0: e.tensor_tensor(out=pp[:, q0:q0 + 128], in0=pp[:, q0:q0 + 128], in1=causal_s, op=ALU.mult),
                              reads=[f"pts{ppi}", "causal"], writes=[f"pts{ppi}"])
                    for qb in range(max(j, 0), 4):
                        last_kc = 4 * Q + qb
                        P.add("pe", lambda e, qb=qb, c=c, pp=pp, vt=vt, kc=kc, last_kc=last_kc: e.matmul(
                            accs[qb][:, c * 256:c * 256 + 129], lhsT=pp[:, qb * 128:(qb + 1) * 128], rhs=vt[:, kc, 0:129],
                            start=(kc == 0), stop=(kc == last_kc)), reads=[f"pts{ppi}", f"vts{vi}"], writes=[acck[qb]])
            for qb in range(4):
                blk = Q * 4 + qb
                acc = accs[qb]
                ak = acck[qb]
                oi, o_t = rot(ot, "ot")
                okk = f"ot{oi}"
                P.add("dve", lambda e, acc=acc: e.reciprocal(small[:, 4:5], acc[:, 128:129]), reads=[ak], writes=["small4"])
                P.add("dve", lambda e, acc=acc: e.reciprocal(small[:, 5:6], acc[:, 384:385]), reads=[ak], writes=["small5"])
                P.add("dve", lambda e: e.tensor_tensor(out=small[:, 5:6], in0=small[:, 5:6], in1=lam_s[:, 2:3], op=ALU.mult),
                      reads=["small5", "lam"], writes=["small5"])
                P.add("dve", lambda e, acc=acc, o_t=o_t: e.tensor_scalar(out=o_t, in0=acc[:, 0:128], scalar1=small[:, 4:5], scalar2=None, op0=ALU.mult),
                      reads=[ak, "small4"], writes=[okk])
                P.add("dve", lambda e, acc=acc, o_t=o_t: e.scalar_tensor_tensor(out=o_t, in0=acc[:, 256:384], scalar=small[:, 5:6], in1=o_t,
                                                                              op0=ALU.mult, op1=ALU.add), reads=[ak, "small5", okk], writes=[okk])
                rms_rstd(o_t, okk, 8, 128, ojunk, "ojunk")
                bi_, obt = rot(ob, "ob")
                P.add("dve", lambda e, o_t=o_t, obt=obt: e.scalar_tensor_tensor(out=obt, in0=o_t, scalar=stats[:, 8:9], in1=subln_s,
                                                                              op0=ALU.mult, op1=ALU.mult), reads=[okk, "stats8", "subln"], writes=[f"ob{bi_}"])
                transpose_blocks(lambda c, obt=obt: obt, f"ob{bi_}", 1, lambda c, h=h, blk=blk: oT[:, 8 + h, blk * 128:(blk + 1) * 128],
                                 lambda c, blk=blk: f"oT{blk}")
    maybe_stop("p13")
    P.barrier()
    R0.reset()
    if debug:
        P.dma("sp", dbg_o.ap().rearrange("c p t -> p c t"), oT, reads=[f"oT{b}" for b in range(NB)], writes=["dbg_o"])

    mT = R0.alloc([128, 16, T], BF16)
    stage = [RF.alloc([128, 512], BF16) for _ in range(3)]
    sga = [RF.alloc([128, 512], BF16) for _ in range(2)]
    sgb = [RF.alloc([128, 512], BF16) for _ in range(2)]
    rt = [RF.alloc([128, 512], F32) for _ in range(3)]
    stagef = [RF.alloc([128, 512], F32) for _ in range(2)]
    P.dma("sp", bcX, mod_d.ap()[2 * D:3 * D].partition_broadcast(128), reads=["mod_d"], writes=["bcX"])

    def ab_load(cg):
        i, wt = rot(wring, "wring")
        P.dma("pool", wt[:, 0:8, :], wa_d[:, cg * 512:(cg + 1) * 512].rearrange("(k p) n -> p k n", p=128), writes=[f"wring{i}"])
        P.dma("pool", wt[:, 8:16, :], wb_d[:, cg * 512:(cg + 1) * 512].rearrange("(k p) n -> p k n", p=128), writes=[f"wring{i}b"])
        return (wt[:, 0:8, :], f"wring{i}"), (wt[:, 8:16, :], f"wring{i}b")

    def ab_compute(cg, cur):
        (wta, wka), (wtb, wkb) = cur
        for blk in range(NB):
            ai, pa = rot(pA, "pA")
            for k in range(8):
                P.add("pe", lambda e, pa=pa, k=k, blk=blk: e.matmul(pa, lhsT=oT[:, k, blk * 128:(blk + 1) * 128], rhs=wta[:, k, :],
                                                                    start=(k == 0), stop=(k == 7)), reads=[f"oT{blk}", wka], writes=[f"pA{ai}"])
            bi, pb_ = rot(pA, "pA")
            for k in range(8):
                P.add("pe", lambda e, pb_=pb_, k=k, blk=blk: e.matmul(pb_, lhsT=oT[:, 8 + k, blk * 128:(blk + 1) * 128], rhs=wtb[:, k, :],
                                                                      start=(k == 0), stop=(k == 7)), reads=[f"oT{blk}", wkb], writes=[f"pA{bi}"])
            gi, ga_t = rot(sga, "sga")
            _, gb_t = rot(sgb, "sgb")
            P.dma("sp", ga_t, sg_d[blk * 128:(blk + 1) * 128, cg * 512:(cg + 1) * 512], reads=["sg_d"], writes=[f"sga{gi}"])
            P.dma("sp", gb_t, sg_d[blk * 128:(blk + 1) * 128, 2048 + cg * 512:2048 + (cg + 1) * 512], reads=["sg_d"], writes=[f"sgb{gi}"])
            P.add("dve", lambda e, pa=pa, ga_t=ga_t: e.tensor_tensor(out=rt[0], in0=pa, in1=ga_t, op=ALU.mult),
                  reads=[f"pA{ai}", f"sga{gi}"], writes=["rt0"])
            P.add("dve", lambda e, pb_=pb_, gb_t=gb_t: e.tensor_tensor(out=rt[1], in0=pb_, in1=gb_t, op=ALU.mult),
                  reads=[f"pA{bi}", f"sgb{gi}"], writes=["rt1"])
            si, st = rot(stage, "stage")
            P.add("pool", lambda e, st=st: e.tensor_tensor(out=st, in0=rt[0], in1=rt[1], op=ALU.add), reads=["rt0", "rt1"], writes=[f"stage{si}"])
            transpose_blocks(lambda c, st=st: st[:, c * 128:(c + 1) * 128], f"stage{si}", 4,
                             lambda c, blk=blk: mT[:, cg * 4 + c, blk * 128:(blk + 1) * 128], lambda c, blk=blk: f"mT{blk}")

    pipelined(list(range(4)), ab_load, ab_compute)

    def wo_compute(dg, cur):
        wt, wk = cur
        for blk in range(NB):
            pi, pt = rot(pA, "pA")
            for k in range(16):
                P.add("pe", lambda e, pt=pt, k=k, blk=blk: e.matmul(pt, lhsT=mT[:, k, blk * 128:(blk + 1) * 128], rhs=wt[:, k, :],
                                                                    start=(k == 0), stop=(k == 15)), reads=[f"mT{blk}", wk], writes=[f"pA{pi}"])
            fi, sf = rot(stagef, "stagef")
            fk = f"stagef{fi}"
            P.dma("sp", sf, x_d[blk * 128:(blk + 1) * 128, dg * 512:(dg + 1) * 512], writes=[fk])
            P.add("dve", lambda e, pt=pt: e.tensor_tensor(out=rt[2], in0=pt, in1=bcX[:, dg * 512:(dg + 1) * 512], op=ALU.mult),
                  reads=[f"pA{pi}", "bcX"], writes=["rt2"])
            P.add("pool", lambda e, sf=sf: e.tensor_tensor(out=sf, in0=sf, in1=rt[2], op=ALU.add), reads=[fk, "rt2"], writes=[fk])
            P.dma("sp", x1_d[blk * 128:(blk + 1) * 128, dg * 512:(dg + 1) * 512], sf, reads=[fk], writes=["x1_d"])

    pipelined(list(range(4)), lambda dg: load_w(wo_d, 16, dg * 512, 512), wo_compute)
    P.barrier()
    RF.reset(RF_mark)
    R0.reset()
    R1.reset()
    if debug:
        P.dma("sp", dbg_x1[:, :], x1_d[:, :], reads=["x1_d"], writes=["dbg_x1"])
    if stop == "p1":
        P.dma("sp", out_d[:, :], x1_d[:, :], reads=["x1_d"], writes=["out_d"])
        P.barrier()
        P.emit()
        es.close()
        return nc

    h2v = R1.alloc([128, NB, D], BF16)
    h2f = R0.alloc([128, D], F32)
    h2T = R0.alloc([128, 16, 128], F32)
    wr_s = R0.alloc([128, 16, 72], F32)
    br_s = R0.alloc([128, 72], F32)
    lg = R0.alloc([128, 72], F32)
    sm = R0.alloc([128, 96], F32)
    E12 = R0.alloc([128, NB, 2, NE], F32)
    cnt_s = R0.alloc([128, NB, NE], F32)
    pos_s = R0.alloc([128, NB, NE], F32)
    tot_s = R0.alloc([128, NB, NE], F32)
    off_s = R0.alloc([128, NB, NE], F32)
    eidx_s = R0.alloc([128, NE], F32)
    tmp64 = R0.alloc([128, NE], F32)
    dsel = R0.alloc([128, NB, 2, 4], F32)
    xt = [RF.alloc([128, D], F32) for _ in range(2)]
    sq_junk = RF.alloc([128, D], BF16)
    gtmp = R0.alloc([128, D], F32)
    P.dma("sp", bcX, mod_d.ap()[4 * D:5 * D].partition_broadcast(128), reads=["mod_d"], writes=["bcX"])
    P.dma("sp", bcY, mod_d.ap()[3 * D:4 * D].partition_broadcast(128), reads=["mod_d"], writes=["bcY"])
    P.dma("sp", gtmp, gffn_d[:, :], writes=["gtmp"])
    P.add("dve", lambda e: e.scalar_tensor_tensor(out=bcX, in0=bcX, scalar=1.0, in1=gtmp, op0=ALU.add, op1=ALU.mult),
          reads=["bcX", "gtmp"], writes=["bcX"])
    P.dma("sp", wr_s, wr_d.ap().rearrange("(k p) n -> p k n", p=128), writes=["wr"])
    P.dma("sp", br_s, br_d[:, :], writes=["br"])
    P.dma("sp", eidx_s, eidx_d[:, :], writes=["eidx"])
    pR = pA[0]
    for blk in range(NB):
        xi, xtile = rot(xt, "xt")
        xk = f"xt{xi}"
        P.dma("sp", xtile, x1_d[blk * 128:(blk + 1) * 128, :], reads=["x1_d"], writes=[xk])
        rms_rstd(xtile, xk, 1, D, sq_junk, "sq_junk")
        P.add("dve", lambda e, xtile=xtile: e.scalar_tensor_tensor(out=h2f, in0=xtile, scalar=stats[:, 1:2], in1=bcX, op0=ALU.mult, op1=ALU.mult),
              reads=[xk, "stats1", "bcX"], writes=["h2f"])
        P.add("pool", lambda e: e.tensor_tensor(out=h2f, in0=h2f, in1=bcY, op=ALU.add), reads=["h2f", "bcY"], writes=["h2f"])
        P.add("act", lambda e, blk=blk: e.copy(h2v[:, blk, :], h2f), reads=["h2f"], writes=[f"h2b{blk}"])
        for c0 in range(0, 16, 4):
            ci, pc = rot(pC, "pC")
            for j in range(4):
                P.add("pe", lambda e, pc=pc, j=j, c=c0 + j: e.transpose(pc[:, j * 128:(j + 1) * 128], h2f[:, c * 128:(c + 1) * 128], ident_f),
                      reads=["h2f", "ident_f"], writes=[f"pC{ci}"])
            P.add("dve", lambda e, pc=pc, c0=c0: e.tensor_copy(h2T[:, c0:c0 + 4, :], pc.rearrange("p (c t) -> p c t", t=128)),
                  reads=[f"pC{ci}"], writes=["h2T"])
        for k in range(16):
            P.add("pe", lambda e, k=k: e.matmul(pR[:, 0:72], lhsT=h2T[:, k, :], rhs=wr_s[:, k, :], start=(k == 0), stop=(k == 15)),
                  reads=["h2T", "wr"], writes=["pA0"])
        P.add("dve", lambda e: e.tensor_tensor(out=lg, in0=pR[:, 0:72], in1=br_s, op=ALU.add), reads=["pA0", "br"], writes=["lg"])

        def dv(fn, reads=("lg", "sm"), writes=("sm",)):
            P.add("dve", fn, reads=list(reads), writes=list(writes))
        dv(lambda e: e.reduce_max(out=sm[:, 0:1], in_=lg[:, 0:8], axis=AX.X))
        dv(lambda e: e.tensor_scalar(out=sm[:, 8:16], in0=lg[:, 0:8], scalar1=sm[:, 0:1], scalar2=None, op0=ALU.is_equal))
        dv(lambda e: e.tensor_scalar(out=sm[:, 32:40], in0=lg[:, 0:8], scalar1=sm[:, 0:1], scalar2=None, op0=ALU.subtract))
        P.add("act", lambda e: e.activation(out=sm[:, 32:40], in_=sm[:, 32:40], func=AF.Exp, accum_out=sm[:, 1:2]), reads=["sm"], writes=["sm"])
        dv(lambda e: e.reciprocal(sm[:, 2:3], sm[:, 1:2]))
        dv(lambda e: e.tensor_tensor(out=sm[:, 32:96].rearrange("p (g e) -> p g e", e=8), in0=lg[:, 8:72].rearrange("p (g e) -> p g e", e=8),
                                     in1=sm[:, 8:16].unsqueeze(2).to_broadcast([128, 8, 8]), op=ALU.mult))
        dv(lambda e: e.reduce_sum(out=sm[:, 16:24], in_=sm[:, 32:96].rearrange("p (g e) -> p e g", e=8), axis=AX.X))
        dv(lambda e: e.reduce_max(out=sm[:, 3:4], in_=sm[:, 16:24], axis=AX.X))
        dv(lambda e: e.tensor_scalar(out=sm[:, 24:32], in0=sm[:, 16:24], scalar1=sm[:, 3:4], scalar2=None, op0=ALU.is_equal))
        dv(lambda e: e.scalar_tensor_tensor(out=sm[:, 32:40], in0=sm[:, 24:32], scalar=-1e30, in1=sm[:, 16:24], op0=ALU.mult, op1=ALU.add))
        dv(lambda e: e.reduce_max(out=sm[:, 4:5], in_=sm[:, 32:40], axis=AX.X))
        dv(lambda e: e.tensor_scalar(out=sm[:, 40:48], in0=sm[:, 32:40], scalar1=sm[:, 4:5], scalar2=None, op0=ALU.is_equal))
        dv(lambda e: e.tensor_tensor(out=sm[:, 5:6], in0=sm[:, 4:5], in1=sm[:, 3:4], op=ALU.subtract))
        P.add("act", lambda e: e.activation(out=sm[:, 5:6], in_=sm[:, 5:6], func=AF.Exp), reads=["sm"], writes=["sm"])
        dv(lambda e: e.tensor_scalar(out=sm[:, 6:7], in0=sm[:, 5:6], scalar1=1.0, scalar2=None, op0=ALU.add))
        dv(lambda e: e.reciprocal(sm[:, 6:7], sm[:, 6:7]))
        dv(lambda e: e.tensor_tensor(out=sm[:, 7:8], in0=sm[:, 5:6], in1=sm[:, 6:7], op=ALU.mult))
        dv(lambda e, blk=blk: e.tensor_scalar(out=wsel2[:, blk, :], in0=sm[:, 6:8], scalar1=sm[:, 2:3], scalar2=None, op0=ALU.mult),
           writes=("sm", "wsel2"))
        for kk, mc in ((0, 24), (1, 40)):
            dv(lambda e, blk=blk, kk=kk, mc=mc: e.tensor_tensor(
                out=E12[:, blk, kk, :].rearrange("p (g e) -> p g e", e=8), in0=sm[:, 8:16].unsqueeze(2).to_broadcast([128, 8, 8]),
                in1=sm[:, mc:mc + 8].unsqueeze(1).to_broadcast([128, 8, 8]), op=ALU.mult), writes=("sm", "E12"))
    P.add("dve", lambda e: e.tensor_tensor(out=cnt_s, in0=E12[:, :, 0, :], in1=E12[:, :, 1, :], op=ALU.add), reads=["E12"], writes=["cnt"])
    cnt2 = cnt_s.rearrange("p b e -> p (b e)")
    for hf in range(2):
        P.add("pe", lambda e, hf=hf: e.matmul(pA[1], lhsT=utri_s, rhs=cnt2[:, hf * 512:(hf + 1) * 512], start=True, stop=True),
              reads=["cnt", "utri"], writes=["pA1"])
        P.add("pe", lambda e, hf=hf: e.matmul(pA[2], lhsT=ones_s, rhs=cnt2[:, hf * 512:(hf + 1) * 512], start=True, stop=True),
              reads=["cnt", "ones"], writes=["pA2"])
        P.add("dve", lambda e, hf=hf: e.tensor_copy(pos_s.rearrange("p b e -> p (b e)")[:, hf * 512:(hf + 1) * 512], pA[1]), reads=["pA1"], writes=["pos_s"])
        P.add("dve", lambda e, hf=hf: e.tensor_copy(tot_s.rearrange("p b e -> p (b e)")[:, hf * 512:(hf + 1) * 512], pA[2]), reads=["pA2"], writes=["tot_s"])
    P.add("pool", lambda e: e.memset(off_s[:, 0, :], 0.0), writes=["off_s"])
    for blk in range(1, NB):
        P.add("dve", lambda e, blk=blk: e.tensor_tensor(out=off_s[:, blk, :], in0=off_s[:, blk - 1, :], in1=tot_s[:, blk - 1, :], op=ALU.add),
              reads=["off_s", "tot_s"], writes=["off_s"])
    P.add("dve", lambda e: e.tensor_tensor(out=pos_s, in0=pos_s, in1=off_s, op=ALU.add), reads=["pos_s", "off_s"], writes=["pos_s"])
    for blk in range(NB):
        for kk in range(2):
            ds = dsel[:, blk, kk, :]
            rk = ["E12", "pos_s", "eidx", "dsel", "tmp64"]
            P.add("dve", lambda e, blk=blk, kk=kk: e.tensor_tensor(out=tmp64, in0=E12[:, blk, kk, :], in1=pos_s[:, blk, :], op=ALU.mult), reads=rk, writes=["tmp64"])
            P.add("dve", lambda e, ds=ds: e.reduce_sum(out=ds[:, 0:1], in_=tmp64, axis=AX.X), reads=rk, writes=["dsel"])
            P.add("dve", lambda e, blk=blk, kk=kk: e.tensor_tensor(out=tmp64, in0=E12[:, blk, kk, :], in1=eidx_s, op=ALU.mult), reads=rk, writes=["tmp64"])
            P.add("dve", lambda e, ds=ds: e.reduce_sum(out=ds[:, 1:2], in_=tmp64, axis=AX.X), reads=rk, writes=["dsel"])
            P.add("dve", lambda e, ds=ds: e.tensor_scalar(out=ds[:, 2:3], in0=ds[:, 0:1], scalar1=float(CAP) - 0.5, scalar2=None, op0=ALU.is_ge),
                  reads=rk, writes=["dsel"])
            P.add("dve", lambda e, ds=ds: e.scalar_tensor_tensor(out=ds[:, 3:4], in0=ds[:, 1:2], scalar=float(CAP), in1=ds[:, 0:1], op0=ALU.mult, op1=ALU.add),
                  reads=rk, writes=["dsel"])
            P.add("dve", lambda e, ds=ds: e.tensor_scalar(out=ds[:, 0:1], in0=ds[:, 3:4], scalar1=-1.0, scalar2=float(NE * CAP), op0=ALU.mult, op1=ALU.add),
                  reads=rk, writes=["dsel"])
            P.add("dve", lambda e, ds=ds: e.tensor_tensor(out=ds[:, 0:1], in0=ds[:, 0:1], in1=ds[:, 2:3], op=ALU.mult), reads=rk, writes=["dsel"])
            P.add("dve", lambda e, ds=ds: e.tensor_tensor(out=ds[:, 3:4], in0=ds[:, 3:4], in1=ds[:, 0:1], op=ALU.add), reads=rk, writes=["dsel"])
            P.add("dve", lambda e, ds=ds, blk=blk, kk=kk: e.tensor_copy(dest_i[:, blk, kk:kk + 1], ds[:, 3:4]), reads=rk, writes=["dest_i"])
    if debug:
        P.dma("sp", dbg_dest[:, :, :, :], dsel, reads=["dsel"], writes=["dbg_dest"])
        P.dma("sp", dbg_w[:, :, :], wsel2, reads=["wsel2"], writes=["dbg_w"])
    for blk in range(NB):
        for kk in range(2):
            P.add("pool", lambda e, blk=blk, kk=kk: e.indirect_dma_start(
                out=xs_d[:, :], out_offset=bass.IndirectOffsetOnAxis(ap=dest_i[:, blk, kk:kk + 1], axis=0), in_=h2v[:, blk, :], in_offset=None),
                reads=[f"h2b{blk}", "dest_i"], writes=["xs_d"], dma=True)
    maybe_stop("p2a")
    P.barrier()
    RF.reset(RF_mark)
    R0.reset()
    R1.reset()

    xs = [R0.alloc([128, NSB, D], BF16) for _ in range(2)]
    xT = R0.alloc([128, 16, CAP], BF16)
    actT = R0.alloc([128, 8, CAP], BF16)
    gsb = [R0.alloc([128, CAP], BF16) for _ in range(2)]
    yst = [R0.alloc([128, D], BF16) for _ in range(2)]
    wdn_t = [R1.alloc([128, 8, 512], BF16) for _ in range(4)]
    wring4 = wring + [R1.alloc([128, 16, 512], BF16) for _ in range(2)]

    def load_gu(ex, fg):
        out = []
        for c0 in (fg * 512, 1024 + fg * 512):
            i, wt = rot(wring4, "wring4")
            key = f"wring{i}"
            P.dma("pool", wt, wgu_d[ex][:, c0:c0 + 512].rearrange("(k p) n -> p k n", p=128), writes=[key])
            out.append((wt, key))
        return out

    def load_expert_x(ex):
        i, t = rot(xs, "xs")
        P.dma("sp", t, xs_d[ex * CAP:(ex + 1) * CAP, :].rearrange("(s p) d -> p s d", p=128), reads=["xs_d"], writes=[f"xs{i}"])
        return t, f"xs{i}"

    P.add("pool", lambda e: e.memset(yst[0], 0.0), writes=["yst0"])
    P.dma("sp", ys_d[NE * CAP:NE * CAP + 128, :], yst[0], reads=["yst0"], writes=["ys_d"])
    nxt_x = load_expert_x(0)
    gu_q = [load_gu(0, 0), load_gu(0, 1)]
    for ex in range(NE):
        xtile, xk = nxt_x
        if ex + 1 < NE:
            nxt_x = load_expert_x(ex + 1)
        wds = []
        for dg in range(4):
            di, wd = rot(wdn_t, "wdn")
            P.dma("pool", wd, wdn_d[ex][:, dg * 512:(dg + 1) * 512].rearrange("(k p) n -> p k n", p=128), writes=[f"wdn{di}"])
            wds.append((wd, f"wdn{di}"))
        for sbk in range(NSB):
            transpose_blocks(lambda c, sbk=sbk: xtile[:, sbk, c * 128:(c + 1) * 128], xk, 16,
                             lambda c, sbk=sbk: xT[:, c, sbk * 128:(sbk + 1) * 128], lambda c: "xT")
        for fg in range(2):
            (wg, wgk), (wu, wuk) = gu_q.pop(0)
            for fc in range(4):
                gi_, pg = rot(pA, "pA")
                for k in range(16):
                    P.add("pe", lambda e, pg=pg, wg=wg, k=k, fc=fc: e.matmul(pg[:, 0:CAP], lhsT=wg[:, k, fc * 128:(fc + 1) * 128], rhs=xT[:, k, :],
                                                                             start=(k == 0), stop=(k == 15)), reads=[wgk, "xT"], writes=[f"pA{gi_}"])
                ui_, pu = rot(pA, "pA")
                for k in range(16):
                    P.add("pe", lambda e, pu=pu, wu=wu, k=k, fc=fc: e.matmul(pu[:, 0:CAP], lhsT=wu[:, k, fc * 128:(fc + 1) * 128], rhs=xT[:, k, :],
                                                                             start=(k == 0), stop=(k == 15)), reads=[wuk, "xT"], writes=[f"pA{ui_}"])
                si_, gs = rot(gsb, "gsb")
                P.add("act", lambda e, gs=gs, pg=pg: e.activation(out=gs, in_=pg[:, 0:CAP], func=AF.Silu), reads=[f"pA{gi_}"], writes=[f"gsb{si_}"])
                P.add("dve", lambda e, gs=gs, pu=pu, fg=fg, fc=fc: e.tensor_tensor(out=actT[:, fg * 4 + fc, :], in0=pu[:, 0:CAP], in1=gs, op=ALU.mult),
                      reads=[f"pA{ui_}", f"gsb{si_}"], writes=["actT"])
            if ex + 1 < NE:
                gu_q.append(load_gu(ex + 1, fg))
        for sbk in range(NSB):
            yi, yt = rot(yst, "yst")
            for dg in range(4):
                wd, wdk = wds[dg]
                pi, pt = rot(pA, "pA")
                for k in range(8):
                    P.add("pe", lambda e, pt=pt, wd=wd, k=k, sbk=sbk: e.matmul(pt, lhsT=actT[:, k, sbk * 128:(sbk + 1) * 128], rhs=wd[:, k, :],
                                                                               start=(k == 0), stop=(k == 7)), reads=["actT", wdk], writes=[f"pA{pi}"])
                P.add("act", lambda e, pt=pt, yt=yt, dg=dg: e.copy(yt[:, dg * 512:(dg + 1) * 512], pt), reads=[f"pA{pi}"], writes=[f"yst{yi}"])
            P.dma("sp", ys_d[ex * CAP + sbk * 128:ex * CAP + (sbk + 1) * 128, :], yt, reads=[f"yst{yi}"], writes=["ys_d"])
    P.barrier()
    RF.reset(RF_mark)
    R0.reset()
    R1.reset()

    xt = [R0.alloc([128, D], F32) for _ in range(2)]
    y1 = [R0.alloc([128, D], BF16) for _ in range(2)]
    y2 = [R0.alloc([128, D], BF16) for _ in range(2)]
    ff = R0.alloc([128, D], F32)
    sq_junk = R0.alloc([128, D], BF16)
    P.dma("sp", bcX, mod_d.ap()[5 * D:6 * D].partition_broadcast(128), reads=["mod_d"], writes=["bcX"])
    P.dma("sp", bcY, gfin_d[:, :], writes=["bcY"])
    for blk in range(NB):
        xi, xtile = rot(xt, "xt")
        xk = f"xt{xi}"
        yi, y1t = rot(y1, "y1")
        _, y2t = rot(y2, "y2")
        P.dma("sp", xtile, x1_d[blk * 128:(blk + 1) * 128, :], reads=["x1_d"], writes=[xk])
        for kk, yt in ((0, y1t), (1, y2t)):
            yk = f"y{kk}_{yi}"
            P.add("pool", lambda e, yt=yt: e.memset(yt, 0.0), writes=[yk])
            P.add("pool", lambda e, yt=yt, blk=blk, kk=kk: e.indirect_dma_start(
                out=yt, out_offset=None, in_=ys_d[:, :], in_offset=bass.IndirectOffsetOnAxis(ap=dest_i[:, blk, kk:kk + 1], axis=0)),
                reads=["ys_d", "dest_i"], writes=[yk], dma=True)
        P.add("dve", lambda e, y1t=y1t, blk=blk: e.tensor_scalar(out=ff, in0=y1t, scalar1=wsel2[:, blk, 0:1], scalar2=None, op0=ALU.mult),
              reads=[f"y0_{yi}", "wsel2"], writes=["ff"])
        P.add("dve", lambda e, y2t=y2t, blk=blk: e.scalar_tensor_tensor(out=ff, in0=y2t, scalar=wsel2[:, blk, 1:2], in1=ff, op0=ALU.mult, op1=ALU.add),
              reads=[f"y1_{yi}", "wsel2", "ff"], writes=["ff"])
        P.add("pool", lambda e: e.tensor_tensor(out=ff, in0=ff, in1=bcX, op=ALU.mult), reads=["ff", "bcX"], writes=["ff"])
        P.add("pool", lambda e, xtile=xtile: e.tensor_tensor(out=xtile, in0=xtile, in1=ff, op=ALU.add), reads=[xk, "ff"], writes=[xk])
        rms_rstd(xtile, xk, 2, D, sq_junk, "sq_junk")
        P.add("dve", lambda e, xtile=xtile: e.scalar_tensor_tensor(out=xtile, in0=xtile, scalar=stats[:, 2:3], in1=bcY, op0=ALU.mult, op1=ALU.mult),
              reads=[xk, "stats2", "bcY"], writes=[xk])
        P.dma("sp", out_d[blk * 128:(blk + 1) * 128, :], xtile, reads=[xk], writes=["out_d"])
    P.barrier()
    P.emit()
    es.close()
    return nc


_NC_CACHE = {}


def make_in_maps(x, c, positions, w_ada, b_ada, g_mix, w_in, swa_sinks, diff_lambda_q1, diff_lambda_k1, diff_lambda_q2,
                 diff_lambda_k2, diff_subln_g, w_branch_a, w_branch_b, w_out, g_ffn, w_router_group, b_router_group,
                 w_router_expert, b_router_expert, w_exp_gate_up, w_exp_down, g_final):
    f32 = np.float32
    A = lambda a: np.ascontiguousarray(np.asarray(a))
    x = A(x); c = A(c); positions = A(positions)
    rep = lambda v: np.ascontiguousarray(np.broadcast_to(np.asarray(v, f32).reshape(1, -1), (128, np.asarray(v).size)))
    fm = lambda v: np.ascontiguousarray(np.asarray(v, f32).reshape(-1, 128).T)
    ident = np.eye(128, dtype=f32)
    p = np.arange(128)
    utri = (p[:, None] < p[None, :]).astype(f32)
    causal = (p[:, None] <= p[None, :]).astype(f32)
    swam = np.concatenate([causal, 1.0 - causal], axis=1)
    invf = (10000.0 ** (-np.arange(0, 64, 2, dtype=f32) / 64.0)).astype(f32)
    shared = {
        "w_ada": A(w_ada)[0], "b_ada": A(b_ada)[0].reshape(1, -1), "g_mixT": fm(A(g_mix)[0]), "w_in": A(w_in)[0],
        "sinks": rep(A(swa_sinks)[0]),
        "lamv": np.ascontiguousarray(np.broadcast_to(np.stack([A(diff_lambda_q1)[0], A(diff_lambda_k1)[0], A(diff_lambda_q2)[0],
                                                                A(diff_lambda_k2)[0]])[None], (128, 4, 64))).astype(f32),
        "subln": rep(A(diff_subln_g)[0]), "w_a": A(w_branch_a)[0], "w_b": A(w_branch_b)[0], "w_o": A(w_out)[0],
        "g_ffn": rep(A(g_ffn)[0]),
        "w_r": np.ascontiguousarray(np.concatenate([A(w_router_group)[0], A(w_router_expert)[0]], axis=1)),
        "b_r": rep(np.concatenate([A(b_router_group)[0], A(b_router_expert)[0]])),
        "w_gu": A(w_exp_gate_up)[0], "w_dn": A(w_exp_down)[0], "g_fin": rep(A(g_final)),
        "ident_b": ident.astype(ml_dtypes.bfloat16), "ident_f": ident,
        "utri": utri, "causal": causal.astype(ml_dtypes.bfloat16), "swamask": swam.astype(ml_dtypes.bfloat16),
        "invfreq": rep((invf.astype(np.float64) / (2.0 * np.pi)).astype(f32)),
        "eidx": rep(np.arange(NE, dtype=f32)),
    }
    in_maps = []
    for b in range(NCORES):
        m = dict(shared)
        m["x"] = x[b]
        m["cT"] = fm(c[b])
        m["pos"] = np.ascontiguousarray(positions[b].reshape(NB, 128).T.astype(np.int32))
        in_maps.append(m)
    return in_maps


def kernel(**inputs):
    in_maps = make_in_maps(**inputs)
    if "nc" not in _NC_CACHE:
        _NC_CACHE["nc"] = build_program()
    res = run_bass_kernel_spmd(_NC_CACHE["nc"], in_maps, core_ids=list(range(NCORES)))
    return np.stack([np.asarray(res.results[b]["out"], dtype=np.float32) for b in range(NCORES)], axis=0)
```

```python
import math
import types
from contextlib import ExitStack
import numpy as np
import ml_dtypes
import concourse.bass as bass
import concourse.mybir as mybir
from concourse.bass_utils import run_bass_kernel_spmd

F32, BF16, I32 = mybir.dt.float32, mybir.dt.bfloat16, mybir.dt.int32
ALU = mybir.AluOpType
AF = mybir.ActivationFunctionType
AX = mybir.AxisListType
DTB = {F32: 4, BF16: 2, I32: 4}

NCORES = 8
T = 2048
D = 2048
NB = T // 128
CAP = 384
NSB = CAP // 128
NE = 64
EPS = 1e-6
NDS = 8
LAMBDA_INIT = 0.8 - 0.6 * math.exp(-0.3 * 0)


def _freeze(fn):
    if fn.__closure__ is None:
        return fn
    cells = []
    for c in fn.__closure__:
        try:
            cells.append(types.CellType(c.cell_contents))
        except ValueError:
            cells.append(c)
    return types.FunctionType(fn.__code__, fn.__globals__, fn.__name__, fn.__defaults__, tuple(cells))


class Prog:
    ENGS = ("pe", "act", "dve", "pool", "sp")

    def __init__(self, nc, es):
        self.nc = nc
        self.ops = []
        self.lastw = {}
        self.readers = {}
        self.esem = {e: es.enter_context(nc.semaphore("s_" + e)) for e in self.ENGS}
        self.dsem = {q: [es.enter_context(nc.semaphore(f"d_{q}_{i}")) for i in range(NDS)]
                     for q in ("sp", "act", "pool")}

    def add(self, eng, fn, reads=(), writes=(), dma=False):
        i = len(self.ops)
        deps = set()
        reads = list(reads) + ["__phase"]
        for k in reads:
            if k in self.lastw:
                deps.add(self.lastw[k])
            if k[:2] in ("pA", "pB", "pC"):
                for r in self.readers.get(k, ()):
                    if self.ops[r]["eng"] != eng:
                        deps.add(r)
        for k in writes:
            if k in self.lastw:
                deps.add(self.lastw[k])
            for r in self.readers.get(k, ()):
                deps.add(r)
        self.ops.append(dict(eng=eng, fn=_freeze(fn), deps=deps, dma=dma))
        for k in reads:
            self.readers.setdefault(k, []).append(i)
        for k in writes:
            self.lastw[k] = i
            self.readers[k] = []
        return i

    def dma(self, q, out, in_, reads=(), writes=(), **kw):
        return self.add(q, lambda e: e.dma_start(out=out, in_=in_, **kw), reads, writes, dma=True)

    def barrier(self):
        i = len(self.ops)
        deps = set(self.readers.get("__phase", ()))
        if "__phase" in self.lastw:
            deps.add(self.lastw["__phase"])
        self.ops.append(dict(eng="sp", fn=lambda e: e.nop(), deps=deps, dma=False))
        self.lastw["__phase"] = i
        self.readers["__phase"] = []

    def emit(self):
        ops = self.ops
        qcount = {q: 0 for q in self.dsem}
        qhist = {q: [] for q in self.dsem}
        for i, op in enumerate(ops):
            if op["dma"]:
                q = op["eng"]
                k = qcount[q]
                qcount[q] += 1
                op["sem"] = self.dsem[q][k % NDS]
                op["val"] = 16 * (k // NDS + 1)
                if k >= NDS:
                    op["deps"].add(qhist[q][k - NDS])
                qhist[q].append(i)

        def skip(od, op):
            return od["eng"] == "pe" and op["eng"] == "pe" and not od["dma"] and not op["dma"]

        needed = [False] * len(ops)
        for i, op in enumerate(ops):
            for d in op["deps"]:
                if not skip(ops[d], op):
                    needed[d] = True
        cnt = {e: 0 for e in self.ENGS}
        for i, op in enumerate(ops):
            if op["dma"]:
                continue
            if needed[i]:
                cnt[op["eng"]] += 1
                op["sem"] = self.esem[op["eng"]]
                op["val"] = cnt[op["eng"]]
        seen = {e: {} for e in self.ENGS}
        per = {e: [] for e in self.ENGS}
        for i, op in enumerate(ops):
            e = op["eng"]
            waits = {}
            for d in sorted(op["deps"]):
                od = ops[d]
                if skip(od, op):
                    continue
                s, v = od["sem"], od["val"]
                key = id(s)
                if seen[e].get(key, 0) >= v:
                    continue
                seen[e][key] = v
                waits[key] = (s, v)
            op["waits"] = list(waits.values())
            op["signal"] = op["dma"] or needed[i]
            per[e].append(op)

        def mk(name):
            def body(e):
                for op in per[name]:
                    for s, v in op["waits"]:
                        e.wait_ge(s, v)
                    ins = op["fn"](e)
                    if op["signal"]:
                        ins.then_inc(op["sem"], 16 if op["dma"] else 1)
            return body

        with self.nc.Block() as block:
            block.tensor(mk("pe"))
            block.scalar(mk("act"))
            block.vector(mk("dve"))
            block.gpsimd(mk("pool"))
            block.sync(mk("sp"))


class Arena:
    def __init__(self, nc, es, name, nbytes):
        self.t = es.enter_context(nc.sbuf_tensor(name, [128, nbytes // 2], BF16))
        self.cap = nbytes
        self.top = 0

    def alloc(self, shape, dt):
        n = 1
        for s in shape[1:]:
            n *= s
        nb = (n * DTB[dt] + 31) // 32 * 32
        assert self.top + nb <= self.cap, (self.top, nb, self.cap, shape)
        v = self.t[0:shape[0], self.top // 2:(self.top + n * DTB[dt]) // 2]
        self.top += nb
        if dt != BF16:
            v = v.bitcast(dt)
        if len(shape) == 3:
            v = v.rearrange("p (a b) -> p a b", b=shape[2])
        elif len(shape) == 4:
            v = v.rearrange("p (a b c) -> p a b c", b=shape[2], c=shape[3])
        return v

    def reset(self, mark=0):
        self.top = mark


def build_program(debug=False, stop=None):
    try:
        return _build_program(debug, stop)
    except StopIteration as e:
        return e.value


def _build_program(debug=False, stop=None):
    nc = bass.Bass("TRN2", target_bir_lowering=False)
    es = ExitStack()

    def din(name, shape, dt=F32):
        return nc.dram_tensor(name, list(shape), dt, kind="ExternalInput")

    x_d = din("x", [T, D])
    cT_d = din("cT", [128, 16])
    pos_d = din("pos", [128, NB], I32)
    wada_d = din("w_ada", [D, 6 * D])
    bada_d = din("b_ada", [1, 6 * D])
    gmixT_d = din("g_mixT", [128, 16])
    win_d = din("w_in", [D, 8448])
    sinks_d = din("sinks", [128, 16])
    lam_d = din("lamv", [128, 4, 64])
    subln_d = din("subln", [128, 128])
    wa_d = din("w_a", [1024, D])
    wb_d = din("w_b", [1024, D])
    wo_d = din("w_o", [D, D])
    gffn_d = din("g_ffn", [128, D])
    wr_d = din("w_r", [D, 72])
    br_d = din("b_r", [128, 72])
    if stop is None or stop == "full":
        wgu_d = din("w_gu", [NE, D, D])
        wdn_d = din("w_dn", [NE, 1024, D])
    gfin_d = din("g_fin", [128, D])
    identb_d = din("ident_b", [128, 128], BF16)
    identf_d = din("ident_f", [128, 128])
    utri_d = din("utri", [128, 128])
    causal_d = din("causal", [128, 128], BF16)
    swam_d = din("swamask", [128, 256], BF16)
    invf_d = din("invfreq", [128, 32])
    eidx_d = din("eidx", [128, NE])
    out_d = nc.dram_tensor("out", [T, D], F32, kind="ExternalOutput")
    if debug:
        dbg_x1 = nc.dram_tensor("dbg_x1", [T, D], F32, kind="ExternalOutput")
        dbg_o = nc.dram_tensor("dbg_o", [16, 128, T], BF16, kind="ExternalOutput")
        dbg_dest = nc.dram_tensor("dbg_dest", [128, 4, NB, 2], F32, kind="ExternalOutput")
        dbg_w = nc.dram_tensor("dbg_w", [128, NB, 2], F32, kind="ExternalOutput")
        dbg_mod = nc.dram_tensor("dbg_mod", [96, 128], F32, kind="ExternalOutput")

    mod_d = nc.dram_tensor("mod_s", [6 * D], F32)
    qT_d = nc.dram_tensor("qT_s", [25, 128, T], BF16)
    v_d = nc.dram_tensor("v_s", [T, 1152], BF16)
    sg_d = nc.dram_tensor("sg_s", [T, 4096], BF16)
    x1_d = nc.dram_tensor("x1_s", [T, D], F32)
    xs_d = nc.dram_tensor("xs_s", [NE * CAP + 128, D], BF16)
    ys_d = nc.dram_tensor("ys_s", [NE * CAP + 128, D], BF16)

    P = Prog(nc, es)
    R0 = Arena(nc, es, "R0", 64 * 1024)
    R1 = Arena(nc, es, "R1", 64 * 1024)
    RP = Arena(nc, es, "RP", 21 * 1024)
    RF = Arena(nc, es, "RF", 52 * 1024)

    def ps(name, shape, dt=F32):
        return es.enter_context(nc.psum_tensor(name, list(shape), dt))


    def maybe_stop(tag):
        if stop == tag:
            P.barrier()
            P.dma("sp", out_d[0:128, :], x_d[0:128, :], writes=["out_d"])
            P.barrier()
            P.emit()
            es.close()
            raise StopIteration(nc)

    pA = [ps(f"pA{i}", [128, 512])[:] for i in range(4)]
    pB = [ps(f"pB{i}", [128, 8, 128], BF16)[:] for i in range(2)]
    pC = [ps(f"pC{i}", [128, 512])[:] for i in range(2)]

    ctr = {}

    def rot(lst, name):
        i = ctr.get(name, 0)
        ctr[name] = i + 1
        return i % len(lst), lst[i % len(lst)]

    ident_b = RP.alloc([128, 128], BF16)
    ident_f = RP.alloc([128, 128], F32)
    utri_s = RP.alloc([128, 128], F32)
    ones_s = RP.alloc([128, 128], F32)
    causal_s = RP.alloc([128, 128], BF16)
    swam_s = RP.alloc([128, 256], BF16)
    modT = RP.alloc([128, 96], F32)
    a1T = RP.alloc([128, 16], F32)
    esink = RP.alloc([128, 16], F32)
    lam_s = RP.alloc([128, 8], F32)
    subln_s = RP.alloc([128, 128], F32)
    stats = RP.alloc([128, 16], F32)
    small = RP.alloc([128, 16], F32)
    bcX = RP.alloc([128, D], F32)
    bcY = RP.alloc([128, D], F32)
    wsel2 = RP.alloc([128, NB, 2], F32)
    dest_i = RP.alloc([128, NB, 2], I32)
    RP_mark = RP.top

    P.dma("sp", ident_b, identb_d[:, :], writes=["ident_b"])
    P.dma("sp", ident_f, identf_d[:, :], writes=["ident_f"])
    P.dma("sp", utri_s, utri_d[:, :], writes=["utri"])
    P.dma("sp", causal_s, causal_d[:, :], writes=["causal"])
    P.dma("sp", swam_s, swam_d[:, :], writes=["swam"])
    P.add("pool", lambda e: e.memset(ones_s, 1.0), writes=["ones"])
    P.dma("sp", subln_s, subln_d[:, :], writes=["subln"])
    P.dma("sp", esink, sinks_d[:, :], writes=["esink"])
    P.add("act", lambda e: e.activation(out=esink, in_=esink, func=AF.Exp), reads=["esink"], writes=["esink"])

    maybe_stop("c0")

    wring = [RF.alloc([128, 16, 512], BF16) for _ in range(2)]
    RF_mark = RF.top

    def load_w(src2d, kc, c0, width):
        i, wt = rot(wring, "wring")
        key = f"wring{i}"
        P.dma("pool", wt[:, 0:kc, 0:width], src2d[:, c0:c0 + width].rearrange("(k p) n -> p k n", p=128), writes=[key, key + "b"])
        return wt, key

    def pipelined(pieces, loader, compute):
        nxt = loader(pieces[0])
        for i, pc in enumerate(pieces):
            cur = nxt
            if i + 1 < len(pieces):
                nxt = loader(pieces[i + 1])
            compute(pc, cur)

    cos_s = R1.alloc([128, NB, 32], F32)
    sin_s = R1.alloc([128, NB, 32], F32)
    pos_i = R1.alloc([128, NB], I32)
    pos_f = R1.alloc([128, NB], F32)
    invf = R1.alloc([128, 32], F32)
    lamv = R1.alloc([128, 4, 64], F32)
    cT_s = R1.alloc([128, 16], F32)
    cact = R1.alloc([128, 16], BF16)
    gmixT = R1.alloc([128, 16], F32)
    brow = [R1.alloc([1, 512], F32) for _ in range(2)]
    mrow = [R1.alloc([1, 512], F32) for _ in range(2)]
    modR = R1.alloc([96, 128], F32)
    rr_i = R1.alloc([128, NB, 32], I32)
    rr_f = R1.alloc([128, NB, 32], F32)
    R1_p10 = R1.top

    P.dma("sp", pos_i, pos_d[:, :], writes=["pos_i"])
    P.dma("sp", invf, invf_d[:, :], writes=["invf"])
    P.add("dve", lambda e: e.tensor_copy(pos_f, pos_i), reads=["pos_i"], writes=["pos_f"])
    for blk in range(NB):
        P.add("dve", lambda e, blk=blk: e.tensor_scalar(out=sin_s[:, blk, :], in0=invf, scalar1=pos_f[:, blk:blk + 1], scalar2=None, op0=ALU.mult),
              reads=["pos_f", "invf"], writes=["sin"])
    P.add("dve", lambda e: e.tensor_scalar(out=cos_s, in0=sin_s, scalar1=0.25, scalar2=None, op0=ALU.add), reads=["sin"], writes=["cos"])
    for tab, nm in ((sin_s, "sin"), (cos_s, "cos")):
        P.add("dve", lambda e, tab=tab: e.tensor_copy(rr_i, tab), reads=[nm], writes=["rr_i"])
        P.add("dve", lambda e, tab=tab: e.tensor_copy(rr_f, rr_i), reads=["rr_i"], writes=["rr_f"])
        P.add("dve", lambda e, tab=tab: e.tensor_tensor(out=tab, in0=tab, in1=rr_f, op=ALU.subtract), reads=[nm, "rr_f"], writes=[nm])
        P.add("act", lambda e, tab=tab: e.activation(out=tab, in_=tab, func=AF.Sin, scale=2.0 * math.pi), reads=[nm], writes=[nm])

    maybe_stop("c1")
    P.dma("sp", lamv, lam_d[:, :, :], writes=["lamv"])
    P.add("dve", lambda e: e.tensor_tensor(out=lamv[:, 0, :], in0=lamv[:, 0, :], in1=lamv[:, 1, :], op=ALU.mult), reads=["lamv"], writes=["lamv"])
    P.add("dve", lambda e: e.tensor_tensor(out=lamv[:, 2, :], in0=lamv[:, 2, :], in1=lamv[:, 3, :], op=ALU.mult), reads=["lamv"], writes=["lamv"])
    P.add("dve", lambda e: e.reduce_sum(out=lam_s[:, 0:1], in_=lamv[:, 0, :], axis=AX.X), reads=["lamv"], writes=["lam"])
    P.add("dve", lambda e: e.reduce_sum(out=lam_s[:, 1:2], in_=lamv[:, 2, :], axis=AX.X), reads=["lamv"], writes=["lam"])
    P.add("act", lambda e: e.activation(out=lam_s[:, 0:2], in_=lam_s[:, 0:2], func=AF.Exp), reads=["lam"], writes=["lam"])
    P.add("dve", lambda e: e.tensor_tensor(out=lam_s[:, 2:3], in0=lam_s[:, 1:2], in1=lam_s[:, 0:1], op=ALU.subtract), reads=["lam"], writes=["lam"])
    P.add("dve", lambda e: e.tensor_scalar(out=lam_s[:, 2:3], in0=lam_s[:, 2:3], scalar1=-LAMBDA_INIT, scalar2=None, op0=ALU.add),
          reads=["lam"], writes=["lam"])
    P.add("dve", lambda e: e.tensor_scalar(out=subln_s, in0=subln_s, scalar1=1.0 - LAMBDA_INIT, scalar2=None, op0=ALU.mult),
          reads=["subln"], writes=["subln"])

    maybe_stop("c2")
    P.dma("sp", cT_s, cT_d[:, :], writes=["cT"])
    P.add("act", lambda e: e.activation(out=cact, in_=cT_s, func=AF.Silu), reads=["cT"], writes=["cact"])
    mod2 = mod_d.ap().rearrange("(a n) -> a n", a=1)

    def ada_compute(g, cur):
        wt, wk = cur
        pi, pt = rot(pA, "pA")
        bi, br = rot(brow, "brow")
        P.dma("sp", br, bada_d[:, g * 512:(g + 1) * 512], writes=[f"brow{bi}"])
        for k in range(16):
            P.add("pe", lambda e, pt=pt, wt=wt, k=k: e.matmul(pt[0:1, :], lhsT=cact[:, k:k + 1], rhs=wt[:, k, :], start=(k == 0), stop=(k == 15)),
                  reads=["cact", wk], writes=[f"pA{pi}"])
        mi, mr = rot(mrow, "mrow")
        P.add("dve", lambda e, pt=pt, mr=mr, br=br: e.tensor_tensor(out=mr, in0=pt[0:1, :], in1=br, op=ALU.add),
              reads=[f"pA{pi}", f"brow{bi}"], writes=[f"mrow{mi}"])
        P.dma("sp", mod2[:, g * 512:(g + 1) * 512], mr, reads=[f"mrow{mi}"], writes=["mod_d"])

    pipelined(list(range(24)), lambda g: load_w(wada_d, 16, g * 512, 512), ada_compute)
    maybe_stop("c3")
    P.dma("sp", modR, mod_d.ap().rearrange("(c p) -> c p", p=128), reads=["mod_d"], writes=["modR"])
    P.add("pe", lambda e: e.transpose(pC[0][:, 0:96], modR, ident_f[0:96, 0:96]), reads=["modR", "ident_f"], writes=["pC0"])
    P.add("dve", lambda e: e.tensor_copy(modT, pC[0][:, 0:96]), reads=["pC0"], writes=["modT"])
    maybe_stop("c4")
    P.dma("sp", gmixT, gmixT_d[:, :], writes=["gmixT"])
    P.add("dve", lambda e: e.scalar_tensor_tensor(out=a1T, in0=modT[:, 16:32], scalar=1.0, in1=gmixT, op0=ALU.add, op1=ALU.mult),
          reads=["modT", "gmixT"], writes=["a1T"])
    maybe_stop("c5")
    if debug:
        P.dma("sp", dbg_mod[:, :], modR, reads=["modR"], writes=["dbg_mod"])

    maybe_stop("p10")

    def rms_rstd(src, key_src, col, n, junk, junk_key):
        P.add("act", lambda e: e.activation(out=junk, in_=src, func=AF.Square, accum_out=stats[:, col:col + 1]),
              reads=[key_src], writes=[junk_key, f"stats{col}"])
        P.add("act", lambda e: e.activation(out=stats[:, col:col + 1], in_=stats[:, col:col + 1], func=AF.Sqrt, scale=1.0 / n, bias=EPS),
              reads=[f"stats{col}"], writes=[f"stats{col}"])
        P.add("dve", lambda e: e.reciprocal(stats[:, col:col + 1], stats[:, col:col + 1]), reads=[f"stats{col}"], writes=[f"stats{col}"])

    def transpose_blocks(src_fn, src_key, nchunks, dst_fn, dst_key_fn, evac=None, rows=128):
        for c0 in range(0, nchunks, 4):
            n = min(4, nchunks - c0)
            bi, pb = rot(pB, "pB")
            for j in range(n):
                src_ap = src_fn(c0 + j)
                P.add("pe", lambda e, pb=pb, j=j, src_ap=src_ap: e.transpose(pb[0:rows, j, :], src_ap, ident_b),
                      reads=[src_key, "ident_b"], writes=[f"pB{bi}"])
            for j in range(n):
                c = c0 + j
                if evac is None:
                    dst_ap = dst_fn(c)
                    P.add("dve", lambda e, pb=pb, j=j, dst_ap=dst_ap: e.tensor_copy(dst_ap, pb[0:rows, j, :]), reads=[f"pB{bi}"],
                          writes=[dst_key_fn(c)])
                else:
                    evac(pb[0:rows, j, :], f"pB{bi}", c)

    hT = R0.alloc([128, 16, T], BF16)
    xt = [R1.alloc([128, D], F32) for _ in range(2)]
    xn = R1.alloc([128, D], BF16)
    sq_junk = R1.alloc([128, D], BF16)
    for blk in range(NB):
        xi, xtile = rot(xt, "xt")
        P.dma("sp", xtile, x_d[blk * 128:(blk + 1) * 128, :], writes=[f"xt{xi}"])
        rms_rstd(xtile, f"xt{xi}", 0, D, sq_junk, "sq_junk")
        P.add("dve", lambda e, xtile=xtile: e.tensor_scalar(out=xn, in0=xtile, scalar1=stats[:, 0:1], scalar2=None, op0=ALU.mult),
              reads=[f"xt{xi}", "stats0"], writes=["xn"])

        def evac_h(psrc, pkey, c, blk=blk):
            P.add("act", lambda e: e.activation(out=hT[:, c, blk * 128:(blk + 1) * 128], in_=psrc, func=AF.Identity,
                                                bias=modT[:, c:c + 1], scale=a1T[:, c:c + 1]),
                  reads=[pkey, "modT", "a1T"], writes=[f"hT{blk}"])
        transpose_blocks(lambda c: xn[:, c * 128:(c + 1) * 128], "xn", 16, None, None, evac=evac_h)
    maybe_stop("p11")
    P.barrier()
    RF.reset(RF_mark)

    stage = [RF.alloc([128, 512], BF16) for _ in range(4)]
    rt = [RF.alloc([128, 256], F32) for _ in range(4)]
    groups = [(0, 512, "q"), (512, 512, "q"), (1024, 256, "kv"), (1280, 512, "q"), (1792, 512, "q"),
              (2304, 512, "q"), (2816, 512, "q"), (3328, 512, "v"), (3840, 512, "v")] + \
             [(4352 + 512 * i, 512, "g") for i in range(8)]

    def qchunk_of(col):
        if col < 1024:
            return col // 128
        if col < 1152:
            return 8
        return 9 + (col - 1280) // 128

    def win_compute(grp, cur):
        c0, width, kind = grp
        wt, wk = cur
        for blk in range(NB):
            pi, pt = rot(pA, "pA")
            pk = f"pA{pi}"
            for k in range(16):
                P.add("pe", lambda e, pt=pt, wt=wt, k=k, blk=blk: e.matmul(pt[:, 0:width], lhsT=hT[:, k, blk * 128:(blk + 1) * 128],
                                                                           rhs=wt[:, k, 0:width], start=(k == 0), stop=(k == 15)),
                      reads=[f"hT{blk}", wk], writes=[pk])
            si, st = rot(stage, "stage")
            sk = f"stage{si}"
            ropew = width if kind == "q" else (128 if kind == "kv" else 0)
            if stop == "p12:kv2":
                ropew = 0
            if ropew:
                nh = ropew // 64
                pv = pt[:, 0:ropew].rearrange("p (h t i) -> p h t i", t=2, i=32)
                sv = st[:, 0:ropew].rearrange("p (h t i) -> p h t i", t=2, i=32)
                cosb = cos_s[:, blk, :].unsqueeze(1).to_broadcast([128, nh, 32])
                sinb = sin_s[:, blk, :].unsqueeze(1).to_broadcast([128, nh, 32])
                r = [rt[q][:, 0:ropew // 2].rearrange("p (h i) -> p h i", i=32) for q in range(4)]
                P.add("dve", lambda e: e.tensor_tensor(out=r[0], in0=pv[:, :, 0, :], in1=cosb, op=ALU.mult), reads=[pk, "cos"], writes=["rt0"])
                P.add("dve", lambda e: e.tensor_tensor(out=r[1], in0=pv[:, :, 1, :], in1=sinb, op=ALU.mult), reads=[pk, "sin"], writes=["rt1"])
                P.add("dve", lambda e: e.tensor_tensor(out=r[2], in0=pv[:, :, 1, :], in1=cosb, op=ALU.mult), reads=[pk, "cos"], writes=["rt2"])
                P.add("dve", lambda e: e.tensor_tensor(out=r[3], in0=pv[:, :, 0, :], in1=sinb, op=ALU.mult), reads=[pk, "sin"], writes=["rt3"])
                P.add("pool", lambda e: e.tensor_tensor(out=sv[:, :, 0, :], in0=r[0], in1=r[1], op=ALU.subtract), reads=["rt0", "rt1"], writes=[sk])
                P.add("pool", lambda e: e.tensor_tensor(out=sv[:, :, 1, :], in0=r[2], in1=r[3], op=ALU.add), reads=["rt2", "rt3"], writes=[sk])
                nch = ropew // 128
                s2i, st2 = rot(stage, "stage")
                s2k = f"stage{s2i}"
                transpose_blocks(lambda c: st[:, c * 128:(c + 1) * 128], sk, nch, lambda c: st2[:, c * 128:(c + 1) * 128], lambda c: s2k)
                qc0 = qchunk_of(c0)
                if nch == 1:
                    P.dma("sp", qT_d[qc0, :, blk * 128:(blk + 1) * 128], st2[:, 0:128], reads=[s2k], writes=["qT_d"])
                else:
                    P.dma("sp", qT_d[qc0:qc0 + nch, :, blk * 128:(blk + 1) * 128].rearrange("c p t -> p c t"),
                          st2[:, 0:nch * 128].rearrange("p (c t) -> p c t", t=128), reads=[s2k], writes=["qT_d"])
                if kind == "kv" and stop != "p12:kv1":
                    s3i, st3 = rot(stage, "stage")
                    P.add("act", lambda e: e.copy(st3[:, 0:128], pt[:, 128:256]), reads=[pk], writes=[f"stage{s3i}"])
                    P.dma("sp", v_d[blk * 128:(blk + 1) * 128, 0:128], st3[:, 0:128], reads=[f"stage{s3i}"], writes=["v_d"])
            elif kind == "kv":
                s3i, st3 = rot(stage, "stage")
                P.add("act", lambda e: e.copy(st3[:, 0:128], pt[:, 128:256]), reads=[pk], writes=[f"stage{s3i}"])
                P.dma("sp", v_d[blk * 128:(blk + 1) * 128, 0:128], st3[:, 0:128], reads=[f"stage{s3i}"], writes=["v_d"])
            elif kind == "v":
                P.add("act", lambda e: e.copy(st, pt), reads=[pk], writes=[sk])
                vc = 128 + (c0 - 3328)
                P.dma("sp", v_d[blk * 128:(blk + 1) * 128, vc:vc + 512], st, reads=[sk], writes=["v_d"])
            else:
                P.add("act", lambda e: e.activation(out=st, in_=pt, func=AF.Sigmoid), reads=[pk], writes=[sk])
                gc = c0 - 4352
                P.dma("sp", sg_d[blk * 128:(blk + 1) * 128, gc:gc + 512], st, reads=[sk], writes=["sg_d"])

    if stop is not None and stop.startswith("p12:"):
        groups = [g_ for g_ in groups if g_[2] == stop[4:6].rstrip("0123456789")][:1]
    pipelined(groups, lambda grp: load_w(win_d, 16, grp[0], grp[1]), win_compute)
    if stop is not None and stop.startswith("p12"):
        maybe_stop(stop)
    P.barrier()
    RF.reset(RF_mark)
    R0.reset()
    R1.reset()

    oT = R1.alloc([128, 16, T], BF16)
    qts = [R0.alloc([128, T], BF16) for _ in range(2)]
    kts = [R0.alloc([128, T], BF16) for _ in range(2)]
    vts = [R0.alloc([128, NB, 130], BF16) for _ in range(2)]
    pts = [R0.alloc([128, 512], BF16) for _ in range(6)]
    osb = R0.alloc([128, NB, 2, 65], F32)
    dn = R0.alloc([128, NB, 2], F32)
    ob_all = R0.alloc([128, NB, 128], BF16)
    asb = [R0.alloc([128, 4, 2, 129], F32) for _ in range(2)]
    rr = R0.alloc([128, 4, 2], F32)
    ss4 = R0.alloc([128, 4], F32)
    o4 = R0.alloc([128, 4, 128], F32)
    t4 = R0.alloc([128, 4, 128], F32)
    sq4 = R0.alloc([128, 4, 128], F32)
    ob4 = R0.alloc([128, 4, 128], BF16)
    for i in range(2):
        P.add("pool", lambda e, i=i: e.memset(vts[i], 1.0), writes=[f"vts{i}"])

    for j in range(8):
        g = j // 4
        qi, qt = rot(qts, "qts")
        ki, kt = rot(kts, "kts")
        vi, vt = rot(vts, "vts")
        P.dma("sp", qt, qT_d[j, :, :], reads=["qT_d"], writes=[f"qts{qi}"])
        P.dma("sp", kt[0:64, :], qT_d[8, 64 * g:64 * g + 64, :], reads=["qT_d"], writes=[f"kts{ki}"])
        P.dma("sp", kt[64:128, :], qT_d[8, 64 * g:64 * g + 64, :], reads=["qT_d"], writes=[f"kts{ki}"])
        P.dma("sp", vt[:, :, 0:64], v_d[:, 64 * g:64 * g + 64].rearrange("(b p) e -> p b e", p=128), reads=["v_d"], writes=[f"vts{vi}"])
        its = [(kc, hh) for kc in range(NB) for hh in range(2)]

        def swa_S(i):
            kc, hh = its[i]
            lo = 64 * hh
            nq = 256 if kc < NB - 1 else 128
            pt = pA[i % 2]
            P.add("pe", lambda e: e.matmul(pt[:, 0:nq], lhsT=kt[lo:lo + 64, kc * 128:(kc + 1) * 128], rhs=qt[lo:lo + 64, kc * 128:kc * 128 + nq],
                                           start=True, stop=True), reads=[f"kts{ki}", f"qts{qi}"], writes=[f"pA{i % 2}"])

        swa_S(0)
        for i, (kc, hh) in enumerate(its):
            if i + 1 < len(its):
                swa_S(i + 1)
            nq = 256 if kc < NB - 1 else 128
            pt = pA[i % 2]
            ppi = 3 * hh + (kc % 3)
            pp = pts[ppi]
            P.add("act", lambda e: e.activation(out=pp[:, 0:nq], in_=pt[:, 0:nq], func=AF.Exp, scale=0.125), reads=[f"pA{i % 2}"], writes=[f"pts{ppi}"])
            P.add("pool", lambda e: e.tensor_tensor(out=pp[:, 0:nq], in0=pp[:, 0:nq], in1=swam_s[:, 0:nq], op=ALU.mult),
                  reads=[f"pts{ppi}", "swam"], writes=[f"pts{ppi}"])
            ci = (kc // 3) % 2
            pc = pC[ci]
            slot = (kc % 3) * 2 + hh
            oc = pc[:, slot * 65:(slot + 1) * 65]
            if kc > 0:
                pppi = 3 * hh + ((kc - 1) % 3)
                ppp = pts[pppi]
                P.add("pe", lambda e: e.matmul(oc, lhsT=ppp[:, 128:256], rhs=vt[:, kc - 1, 0:65], start=True, stop=False),
                      reads=[f"pts{pppi}", f"vts{vi}"], writes=[f"pC{ci}"])
            P.add("pe", lambda e: e.matmul(oc, lhsT=pp[:, 0:128], rhs=vt[:, kc, 0:65], start=(kc == 0), stop=True),
                  reads=[f"pts{ppi}", f"vts{vi}"], writes=[f"pC{ci}"])
            if hh == 1 and (kc % 3 == 2 or kc == NB - 1):
                kc0 = 3 * (kc // 3)
                nk = kc - kc0 + 1
                P.add("dve", lambda e: e.tensor_copy(osb[:, kc0:kc0 + nk, :, :], pc[:, 0:nk * 130].rearrange("p (k h n) -> p k h n", h=2, n=65)),
                      reads=[f"pC{ci}"], writes=["osb"])
        for hh in range(2):
            P.add("dve", lambda e, hh=hh: e.tensor_scalar(out=dn[:, :, hh], in0=osb[:, :, hh, 64], scalar1=esink[:, 2 * j + hh:2 * j + hh + 1], scalar2=None,
                                                          op0=ALU.add), reads=["osb", "esink"], writes=["dn"])
        P.add("dve", lambda e: e.reciprocal(dn, dn), reads=["dn"], writes=["dn"])
        P.add("dve", lambda e: e.tensor_tensor(out=ob_all.rearrange("p b (h n) -> p b h n", n=64), in0=osb[:, :, :, 0:64],
                                               in1=dn.unsqueeze(3).to_broadcast([128, NB, 2, 64]), op=ALU.mult), reads=["osb", "dn"], writes=["ob_all"])
        for g8 in range(2):
            bi, pb = rot(pB, "pB")
            for k8 in range(8):
                kc = g8 * 8 + k8
                P.add("pe", lambda e, pb=pb, k8=k8, kc=kc: e.transpose(pb[:, k8, :], ob_all[:, kc, :], ident_b), reads=["ob_all", "ident_b"], writes=[f"pB{bi}"])
            P.add("dve", lambda e, pb=pb, g8=g8: e.tensor_copy(oT[:, j, g8 * 1024:(g8 + 1) * 1024].rearrange("p (k t) -> p k t", t=128), pb),
                  reads=[f"pB{bi}"], writes=[f"oTa{j}"])

    maybe_stop("p13a")
    accs = [pA[2], pA[3], pC[0], pC[1]]
    acck = ["pA2", "pA3", "pC0", "pC1"]
    for h in range(8):
        qi, qt = rot(qts, "qts")
        ki, kt = rot(kts, "kts")
        vi, vt = rot(vts, "vts")
        P.dma("sp", qt, qT_d[9 + h, :, :], reads=["qT_d"], writes=[f"qts{qi}"])
        P.dma("sp", kt, qT_d[17 + h, :, :], reads=["qT_d"], writes=[f"kts{ki}"])
        P.dma("sp", vt[:, :, 0:128], v_d[:, 128 + 128 * h:256 + 128 * h].rearrange("(b p) e -> p b e", p=128), reads=["v_d"], writes=[f"vts{vi}"])
        its = [(Q, c, kc) for Q in range(4) for c in range(2) for kc in range(4 * Q + 4)]

        def diff_S(i):
            Q, c, kc = its[i]
            lo = 64 * c
            jj = kc - 4 * Q
            q0 = 128 * jj if jj > 0 else 0
            pt = pA[i % 2]
            P.add("pe", lambda e: e.matmul(pt[:, q0:512], lhsT=kt[lo:lo + 64, kc * 128:(kc + 1) * 128], rhs=qt[lo:lo + 64, Q * 512 + q0:(Q + 1) * 512],
                                           start=True, stop=True), reads=[f"kts{ki}", f"qts{qi}"], writes=[f"pA{i % 2}"])

        diff_S(0)
        for i, (Q, c, kc) in enumerate(its):
            if i + 1 < len(its):
                diff_S(i + 1)
            jj = kc - 4 * Q
            q0 = 128 * jj if jj > 0 else 0
            pt = pA[i % 2]
            ppi, pp = rot(pts, "pts")
            P.add("act", lambda e: e.activation(out=pp[:, q0:512], in_=pt[:, q0:512], func=AF.Exp, scale=0.125), reads=[f"pA{i % 2}"], writes=[f"pts{ppi}"])
            if jj >= 0:
                P.add("pool", lambda e: e.tensor_tensor(out=pp[:, q0:q0 + 128], in0=pp[:, q0:q0 + 128], in1=causal_s, op=ALU.mult),
                      reads=[f"pts{ppi}", "causal"], writes=[f"pts{ppi}"])
            for qb in range(max(jj, 0), 4):
                last_kc = 4 * Q + qb
                P.add("pe", lambda e, qb=qb, last_kc=last_kc: e.matmul(accs[qb][:, c * 256:c * 256 + 129], lhsT=pp[:, qb * 128:(qb + 1) * 128],
                                                                       rhs=vt[:, kc, 0:129], start=(kc == 0), stop=(kc == last_kc)),
                      reads=[f"pts{ppi}", f"vts{vi}"], writes=[acck[qb]])
            if c == 1 and kc == 4 * Q + 3:
                ai, ab = rot(asb, "asb")
                ak = f"asb{ai}"
                for qb in range(4):
                    P.add("dve", lambda e, qb=qb: e.tensor_copy(ab[:, qb, :, :], accs[qb].rearrange("p (c n) -> p c n", n=256)[:, :, 0:129]),
                          reads=[acck[qb]], writes=[ak])
                P.add("dve", lambda e: e.reciprocal(rr, ab[:, :, :, 128]), reads=[ak], writes=["rr"])
                P.add("dve", lambda e: e.tensor_scalar(out=rr[:, :, 1], in0=rr[:, :, 1], scalar1=lam_s[:, 2:3], scalar2=None, op0=ALU.mult),
                      reads=["rr", "lam"], writes=["rr"])
                P.add("dve", lambda e: e.tensor_tensor(out=o4, in0=ab[:, :, 0, 0:128], in1=rr[:, :, 0:1].to_broadcast([128, 4, 128]), op=ALU.mult),
                      reads=[ak, "rr"], writes=["o4"])
                P.add("dve", lambda e: e.tensor_tensor(out=t4, in0=ab[:, :, 1, 0:128], in1=rr[:, :, 1:2].to_broadcast([128, 4, 128]), op=ALU.mult),
                      reads=[ak, "rr"], writes=["t4"])
                P.add("pool", lambda e: e.tensor_tensor(out=o4, in0=o4, in1=t4, op=ALU.add), reads=["o4", "t4"], writes=["o4"])
                P.add("pool", lambda e: e.tensor_tensor(out=sq4, in0=o4, in1=o4, op=ALU.mult), reads=["o4"], writes=["sq4"])
                P.add("dve", lambda e: e.reduce_sum(out=ss4, in_=sq4, axis=AX.X), reads=["sq4"], writes=["ss4"])
                P.add("act", lambda e: e.activation(out=ss4, in_=ss4, func=AF.Sqrt, scale=1.0 / 128, bias=EPS), reads=["ss4"], writes=["ss4"])
                P.add("dve", lambda e: e.reciprocal(ss4, ss4), reads=["ss4"], writes=["ss4"])
                P.add("dve", lambda e: e.tensor_tensor(out=o4, in0=o4, in1=ss4.unsqueeze(2).to_broadcast([128, 4, 128]), op=ALU.mult),
                      reads=["o4", "ss4"], writes=["o4"])
                P.add("pool", lambda e: e.tensor_tensor(out=ob4, in0=o4, in1=subln_s.unsqueeze(1).to_broadcast([128, 4, 128]), op=ALU.mult),
                      reads=["o4", "subln"], writes=["ob4"])
                bi, pb = rot(pB, "pB")
                for qb in range(4):
                    P.add("pe", lambda e, qb=qb, pb=pb: e.transpose(pb[:, qb, :], ob4[:, qb, :], ident_b), reads=["ob4", "ident_b"], writes=[f"pB{bi}"])
                P.add("dve", lambda e, pb=pb: e.tensor_copy(oT[:, 8 + h, Q * 512:(Q + 1) * 512].rearrange("p (k t) -> p k t", t=128), pb[:, 0:4, :]),
                      reads=[f"pB{bi}"], writes=[f"oTb{h}_{Q}"])
    maybe_stop("p13")
    P.barrier()
    R0.reset()
    if debug:
        P.dma("sp", dbg_o.ap().rearrange("c p t -> p c t"), oT, reads=[], writes=["dbg_o"])

    mT = R0.alloc([128, 16, T], BF16)
    stage = [RF.alloc([128, 512], BF16) for _ in range(3)]
    sga = [RF.alloc([128, 512], BF16) for _ in range(2)]
    sgb = [RF.alloc([128, 512], BF16) for _ in range(2)]
    rt = [RF.alloc([128, 512], F32) for _ in range(3)]
    stagef = [RF.alloc([128, 512], F32) for _ in range(2)]
    P.dma("sp", bcX, mod_d.ap()[2 * D:3 * D].partition_broadcast(128), reads=["mod_d"], writes=["bcX"])

    def ab_load(cg):
        i, wt = rot(wring, "wring")
        P.dma("pool", wt[:, 0:8, :], wa_d[:, cg * 512:(cg + 1) * 512].rearrange("(k p) n -> p k n", p=128), writes=[f"wring{i}"])
        P.dma("pool", wt[:, 8:16, :], wb_d[:, cg * 512:(cg + 1) * 512].rearrange("(k p) n -> p k n", p=128), writes=[f"wring{i}b"])
        return (wt[:, 0:8, :], f"wring{i}"), (wt[:, 8:16, :], f"wring{i}b")

    def ab_compute(cg, cur):
        (wta, wka), (wtb, wkb) = cur
        for blk in range(NB):
            ai, pa = rot(pA, "pA")
            for k in range(8):
                P.add("pe", lambda e, pa=pa, k=k, blk=blk: e.matmul(pa, lhsT=oT[:, k, blk * 128:(blk + 1) * 128], rhs=wta[:, k, :],
                                                                    start=(k == 0), stop=(k == 7)), reads=[f"oT{blk}", wka], writes=[f"pA{ai}"])
            bi, pb_ = rot(pA, "pA")
            for k in range(8):
                P.add("pe", lambda e, pb_=pb_, k=k, blk=blk: e.matmul(pb_, lhsT=oT[:, 8 + k, blk * 128:(blk + 1) * 128], rhs=wtb[:, k, :],
                                                                      start=(k == 0), stop=(k == 7)), reads=[f"oT{blk}", wkb], writes=[f"pA{bi}"])
            gi, ga_t = rot(sga, "sga")
            _, gb_t = rot(sgb, "sgb")
            P.dma("sp", ga_t, sg_d[blk * 128:(blk + 1) * 128, cg * 512:(cg + 1) * 512], reads=["sg_d"], writes=[f"sga{gi}"])
            P.dma("sp", gb_t, sg_d[blk * 128:(blk + 1) * 128, 2048 + cg * 512:2048 + (cg + 1) * 512], reads=["sg_d"], writes=[f"sgb{gi}"])
            P.add("dve", lambda e, pa=pa, ga_t=ga_t: e.tensor_tensor(out=rt[0], in0=pa, in1=ga_t, op=ALU.mult),
                  reads=[f"pA{ai}", f"sga{gi}"], writes=["rt0"])
            P.add("dve", lambda e, pb_=pb_, gb_t=gb_t: e.tensor_tensor(out=rt[1], in0=pb_, in1=gb_t, op=ALU.mult),
                  reads=[f"pA{bi}", f"sgb{gi}"], writes=["rt1"])
            si, st = rot(stage, "stage")
            P.add("pool", lambda e, st=st: e.tensor_tensor(out=st, in0=rt[0], in1=rt[1], op=ALU.add), reads=["rt0", "rt1"], writes=[f"stage{si}"])
            transpose_blocks(lambda c, st=st: st[:, c * 128:(c + 1) * 128], f"stage{si}", 4,
                             lambda c, blk=blk: mT[:, cg * 4 + c, blk * 128:(blk + 1) * 128], lambda c, blk=blk: f"mT{blk}")

    pipelined(list(range(4)), ab_load, ab_compute)

    def wo_compute(dg, cur):
        wt, wk = cur
        for blk in range(NB):
            pi, pt = rot(pA, "pA")
            for k in range(16):
                P.add("pe", lambda e, pt=pt, k=k, blk=blk: e.matmul(pt, lhsT=mT[:, k, blk * 128:(blk + 1) * 128], rhs=wt[:, k, :],
                                                                    start=(k == 0), stop=(k == 15)), reads=[f"mT{blk}", wk], writes=[f"pA{pi}"])
            fi, sf = rot(stagef, "stagef")
            fk = f"stagef{fi}"
            P.dma("sp", sf, x_d[blk * 128:(blk + 1) * 128, dg * 512:(dg + 1) * 512], writes=[fk])
            P.add("dve", lambda e, pt=pt: e.tensor_tensor(out=rt[2], in0=pt, in1=bcX[:, dg * 512:(dg + 1) * 512], op=ALU.mult),
                  reads=[f"pA{pi}", "bcX"], writes=["rt2"])
            P.add("pool", lambda e, sf=sf: e.tensor_tensor(out=sf, in0=sf, in1=rt[2], op=ALU.add), reads=[fk, "rt2"], writes=[fk])
            P.dma("sp", x1_d[blk * 128:(blk + 1) * 128, dg * 512:(dg + 1) * 512], sf, reads=[fk], writes=["x1_d"])

    pipelined(list(range(4)), lambda dg: load_w(wo_d, 16, dg * 512, 512), wo_compute)
    P.barrier()
    RF.reset(RF_mark)
    R0.reset()
    R1.reset()
    if debug:
        P.dma("sp", dbg_x1[:, :], x1_d[:, :], reads=["x1_d"], writes=["dbg_x1"])
    if stop == "p1":
        P.dma("sp", out_d[:, :], x1_d[:, :], reads=["x1_d"], writes=["out_d"])
        P.barrier()
        P.emit()
        es.close()
        return nc

    h2v = R1.alloc([128, NB, D], BF16)
    h2f = [R0.alloc([128, D], F32) for _ in range(2)]
    h2T = [R0.alloc([128, 16, 128], F32) for _ in range(1)]
    wr_s = R0.alloc([128, 16, 72], F32)
    br_s = R0.alloc([128, 72], F32)
    lg = R0.alloc([128, NB, 72], F32)
    gmask = R0.alloc([128, NB, 8], F32)
    gex = R0.alloc([128, NB, 8], F32)
    esel = R0.alloc([128, NB, 8], F32)
    mask1 = R0.alloc([128, NB, 8], F32)
    mask2 = R0.alloc([128, NB, 8], F32)
    msk = R0.alloc([128, NB, 8], F32)
    sc = R0.alloc([128, 8, NB], F32)
    E12 = R0.alloc([128, NB, 2, NE], F32)
    gtmp = R0.alloc([128, D], F32)
    cnt_s = gtmp[:, 0:1024].rearrange("p (b e) -> p b e", e=NE)
    pos_s = gtmp[:, 1024:2048].rearrange("p (b e) -> p b e", e=NE)
    off_s = R0.alloc([128, NB, NE], F32)
    eidx_s = R0.alloc([128, NE], F32)
    tmp64 = R0.alloc([128, NB, NE], F32)
    sel = tmp64
    tot_s = tmp64
    dsel = R0.alloc([128, 4, NB, 2], F32)
    xt = [RF.alloc([128, D], F32) for _ in range(2)]
    sq_junk = RF.alloc([128, D], BF16)
    P.dma("sp", bcX, mod_d.ap()[4 * D:5 * D].partition_broadcast(128), reads=["mod_d"], writes=["bcX"])
    P.dma("sp", bcY, mod_d.ap()[3 * D:4 * D].partition_broadcast(128), reads=["mod_d"], writes=["bcY"])
    P.dma("sp", gtmp, gffn_d[:, :], writes=["gtmp"])
    P.add("dve", lambda e: e.scalar_tensor_tensor(out=bcX, in0=bcX, scalar=1.0, in1=gtmp, op0=ALU.add, op1=ALU.mult),
          reads=["bcX", "gtmp"], writes=["bcX"])
    P.dma("sp", wr_s, wr_d.ap().rearrange("(k p) n -> p k n", p=128), writes=["wr"])
    P.dma("sp", br_s, br_d[:, :], writes=["br"])
    P.dma("sp", eidx_s, eidx_d[:, :], writes=["eidx"])
    for blk in range(NB):
        xi, xtile = rot(xt, "xt")
        xk = f"xt{xi}"
        hi, hf = rot(h2f, "h2f")
        hk = f"h2f{hi}"
        hT_ = h2T[0]
        hTk = "h2T0"
        P.dma("sp", xtile, x1_d[blk * 128:(blk + 1) * 128, :], reads=["x1_d"], writes=[xk])
        rms_rstd(xtile, xk, 1, D, sq_junk, "sq_junk")
        P.add("dve", lambda e: e.scalar_tensor_tensor(out=hf, in0=xtile, scalar=stats[:, 1:2], in1=bcX, op0=ALU.mult, op1=ALU.mult),
              reads=[xk, "stats1", "bcX"], writes=[hk])
        P.add("pool", lambda e: e.tensor_tensor(out=hf, in0=hf, in1=bcY, op=ALU.add), reads=[hk, "bcY"], writes=[hk])
        P.add("act", lambda e: e.copy(h2v[:, blk, :], hf), reads=[hk], writes=[f"h2b{blk}"])
        for c0 in range(0, 16, 4):
            ci, pc = rot(pC, "pC")
            for j in range(4):
                P.add("pe", lambda e, j=j, c=c0 + j: e.transpose(pc[:, j * 128:(j + 1) * 128], hf[:, c * 128:(c + 1) * 128], ident_f),
                      reads=[hk, "ident_f"], writes=[f"pC{ci}"])
            P.add("dve", lambda e: e.tensor_copy(hT_[:, c0:c0 + 4, :], pc.rearrange("p (c t) -> p c t", t=128)), reads=[f"pC{ci}"], writes=[hTk])
        ri = blk % 2
        pR = pA[ri]
        for k in range(16):
            P.add("pe", lambda e, k=k: e.matmul(pR[:, 0:72], lhsT=hT_[:, k, :], rhs=wr_s[:, k, :], start=(k == 0), stop=(k == 15)),
                  reads=[hTk, "wr"], writes=[f"pA{ri}"])
        P.add("dve", lambda e: e.tensor_tensor(out=lg[:, blk, :], in0=pR[:, 0:72], in1=br_s, op=ALU.add), reads=[f"pA{ri}", "br"], writes=["lg"])

    RK = ["lg", "rt_", "tmp64"]

    def dv(fn, eng="dve"):
        P.add(eng, fn, reads=RK, writes=["rt_", "tmp64"])
    glog = lg[:, :, 0:8]
    elog4 = lg[:, :, 8:72].rearrange("p b (g e) -> p b g e", e=8)

    def bc8(row):
        return row.unsqueeze(2).to_broadcast([128, NB, 8])
    dv(lambda e: e.reduce_max(out=sc[:, 0, :], in_=glog, axis=AX.X))
    dv(lambda e: e.tensor_tensor(out=gmask, in0=glog, in1=bc8(sc[:, 0, :]), op=ALU.is_equal))
    dv(lambda e: e.tensor_tensor(out=gex, in0=glog, in1=bc8(sc[:, 0, :]), op=ALU.subtract))
    dv(lambda e: e.activation(out=gex, in_=gex, func=AF.Exp), eng="act")
    dv(lambda e: e.reduce_sum(out=sc[:, 1, :], in_=gex, axis=AX.X))
    dv(lambda e: e.reciprocal(sc[:, 1, :], sc[:, 1, :]))
    dv(lambda e: e.tensor_tensor(out=sel.rearrange("p b (g e) -> p b g e", e=8), in0=elog4,
                                 in1=gmask.unsqueeze(3).to_broadcast([128, NB, 8, 8]), op=ALU.mult))
    dv(lambda e: e.reduce_sum(out=esel, in_=sel.rearrange("p b (g e) -> p b e g", e=8), axis=AX.X))
    dv(lambda e: e.reduce_max(out=sc[:, 2, :], in_=esel, axis=AX.X))
    dv(lambda e: e.tensor_tensor(out=mask1, in0=esel, in1=bc8(sc[:, 2, :]), op=ALU.is_equal))
    dv(lambda e: e.scalar_tensor_tensor(out=msk, in0=mask1, scalar=-1e30, in1=esel, op0=ALU.mult, op1=ALU.add))
    dv(lambda e: e.reduce_max(out=sc[:, 3, :], in_=msk, axis=AX.X))
    dv(lambda e: e.tensor_tensor(out=mask2, in0=msk, in1=bc8(sc[:, 3, :]), op=ALU.is_equal))
    dv(lambda e: e.tensor_tensor(out=sc[:, 4, :], in0=sc[:, 3, :], in1=sc[:, 2, :], op=ALU.subtract))
    dv(lambda e: e.activation(out=sc[:, 4, :], in_=sc[:, 4, :], func=AF.Exp), eng="act")
    dv(lambda e: e.tensor_scalar(out=sc[:, 5, :], in0=sc[:, 4, :], scalar1=1.0, scalar2=None, op0=ALU.add))
    dv(lambda e: e.reciprocal(sc[:, 5, :], sc[:, 5, :]))
    dv(lambda e: e.tensor_tensor(out=sc[:, 6, :], in0=sc[:, 4, :], in1=sc[:, 5, :], op=ALU.mult))
    P.add("dve", lambda e: e.tensor_tensor(out=wsel2[:, :, 0], in0=sc[:, 5, :], in1=sc[:, 1, :], op=ALU.mult), reads=RK, writes=["wsel2"])
    P.add("dve", lambda e: e.tensor_tensor(out=wsel2[:, :, 1], in0=sc[:, 6, :], in1=sc[:, 1, :], op=ALU.mult), reads=RK + ["wsel2"], writes=["wsel2"])
    for kk, mk_ in ((0, mask1), (1, mask2)):
        P.add("dve", lambda e, kk=kk, mk_=mk_: e.tensor_tensor(
            out=E12[:, :, kk, :].rearrange("p b (g e) -> p b g e", e=8), in0=gmask.unsqueeze(3).to_broadcast([128, NB, 8, 8]),
            in1=mk_.unsqueeze(2).to_broadcast([128, NB, 8, 8]), op=ALU.mult), reads=RK + ["E12"], writes=["E12"])
    P.add("dve", lambda e: e.tensor_tensor(out=cnt_s, in0=E12[:, :, 0, :], in1=E12[:, :, 1, :], op=ALU.add), reads=["E12", "gtmp"], writes=["cnt", "gtmp"])
    cnt2 = cnt_s.rearrange("p b e -> p (b e)")
    for hf_ in range(2):
        P.add("pe", lambda e, hf_=hf_: e.matmul(pA[2], lhsT=utri_s, rhs=cnt2[:, hf_ * 512:(hf_ + 1) * 512], start=True, stop=True),
              reads=["cnt", "utri"], writes=["pA2"])
        P.add("pe", lambda e, hf_=hf_: e.matmul(pA[3], lhsT=ones_s, rhs=cnt2[:, hf_ * 512:(hf_ + 1) * 512], start=True, stop=True),
              reads=["cnt", "ones"], writes=["pA3"])
        P.add("dve", lambda e, hf_=hf_: e.tensor_copy(pos_s.rearrange("p b e -> p (b e)")[:, hf_ * 512:(hf_ + 1) * 512], pA[2]), reads=["pA2", "gtmp"], writes=["pos_s"])
        P.add("dve", lambda e, hf_=hf_: e.tensor_copy(tot_s.rearrange("p b e -> p (b e)")[:, hf_ * 512:(hf_ + 1) * 512], pA[3]), reads=["pA3"], writes=["tmp64"])
    P.add("pool", lambda e: e.memset(off_s[:, 0, :], 0.0), writes=["off_s"])
    for blk in range(1, NB):
        P.add("dve", lambda e, blk=blk: e.tensor_tensor(out=off_s[:, blk, :], in0=off_s[:, blk - 1, :], in1=tot_s[:, blk - 1, :], op=ALU.add),
              reads=["off_s", "tmp64"], writes=["off_s"])
    P.add("dve", lambda e: e.tensor_tensor(out=pos_s, in0=pos_s, in1=off_s, op=ALU.add), reads=["pos_s", "off_s"], writes=["pos_s"])
    rk = ["E12", "pos_s", "eidx", "dsel", "tmp64"]
    for kk in range(2):
        P.add("dve", lambda e, kk=kk: e.tensor_tensor(out=tmp64, in0=E12[:, :, kk, :], in1=pos_s, op=ALU.mult), reads=rk, writes=["tmp64"])
        P.add("dve", lambda e, kk=kk: e.reduce_sum(out=dsel[:, 0, :, kk], in_=tmp64, axis=AX.X), reads=rk, writes=["dsel"])
        P.add("dve", lambda e, kk=kk: e.tensor_tensor(out=tmp64, in0=E12[:, :, kk, :], in1=eidx_s.unsqueeze(1).to_broadcast([128, NB, NE]), op=ALU.mult),
              reads=rk, writes=["tmp64"])
        P.add("dve", lambda e, kk=kk: e.reduce_sum(out=dsel[:, 1, :, kk], in_=tmp64, axis=AX.X), reads=rk, writes=["dsel"])
    d0, d1, d2, d3 = (dsel[:, q, :, :] for q in range(4))
    P.add("dve", lambda e: e.tensor_scalar(out=d2, in0=d0, scalar1=float(CAP) - 0.5, scalar2=None, op0=ALU.is_ge), reads=rk, writes=["dsel"])
    P.add("dve", lambda e: e.scalar_tensor_tensor(out=d3, in0=d1, scalar=float(CAP), in1=d0, op0=ALU.mult, op1=ALU.add), reads=rk, writes=["dsel"])
    P.add("dve", lambda e: e.tensor_scalar(out=d0, in0=d3, scalar1=-1.0, scalar2=float(NE * CAP), op0=ALU.mult, op1=ALU.add), reads=rk, writes=["dsel"])
    P.add("dve", lambda e: e.tensor_tensor(out=d0, in0=d0, in1=d2, op=ALU.mult), reads=rk, writes=["dsel"])
    P.add("dve", lambda e: e.tensor_tensor(out=d3, in0=d3, in1=d0, op=ALU.add), reads=rk, writes=["dsel"])
    P.add("dve", lambda e: e.tensor_copy(dest_i, d3), reads=rk, writes=["dest_i"])
    if debug:
        P.dma("sp", dbg_dest[:, :, :, :], dsel, reads=["dsel"], writes=["dbg_dest"])
        P.dma("sp", dbg_w[:, :, :], wsel2, reads=["wsel2"], writes=["dbg_w"])
    for blk in range(NB):
        for kk in range(2):
            P.add("pool", lambda e, blk=blk, kk=kk: e.indirect_dma_start(
                out=xs_d[:, :], out_offset=bass.IndirectOffsetOnAxis(ap=dest_i[:, blk, kk:kk + 1], axis=0), in_=h2v[:, blk, :], in_offset=None),
                reads=[f"h2b{blk}", "dest_i"], writes=["xs_d"], dma=True)
    maybe_stop("p2a")
    P.barrier()
    RF.reset(RF_mark)
    R0.reset()
    R1.reset()

    xs = [R0.alloc([128, NSB, D], BF16) for _ in range(2)]
    xT = R0.alloc([128, 16, CAP], BF16)
    actT = R0.alloc([128, 8, CAP], BF16)
    gsb = [R0.alloc([128, CAP], BF16) for _ in range(2)]
    yst = [R0.alloc([128, D], BF16) for _ in range(2)]
    wdn_t = [R1.alloc([128, 8, 512], BF16) for _ in range(4)]
    wring4 = wring + [R1.alloc([128, 16, 512], BF16) for _ in range(2)]

    def load_gu(ex, fg):
        out = []
        for c0 in (fg * 512, 1024 + fg * 512):
            i, wt = rot(wring4, "wring4")
            key = f"wring{i}"
            P.dma("pool", wt, wgu_d[ex][:, c0:c0 + 512].rearrange("(k p) n -> p k n", p=128), writes=[key])
            out.append((wt, key))
        return out

    def load_expert_x(ex):
        i, t = rot(xs, "xs")
        P.dma("sp", t, xs_d[ex * CAP:(ex + 1) * CAP, :].rearrange("(s p) d -> p s d", p=128), reads=["xs_d"], writes=[f"xs{i}"])
        return t, f"xs{i}"

    P.add("pool", lambda e: e.memset(yst[0], 0.0), writes=["yst0"])
    P.dma("sp", ys_d[NE * CAP:NE * CAP + 128, :], yst[0], reads=["yst0"], writes=["ys_d"])
    nxt_x = load_expert_x(0)
    gu_q = [load_gu(0, 0), load_gu(0, 1)]
    for ex in range(NE):
        xtile, xk = nxt_x
        if ex + 1 < NE:
            nxt_x = load_expert_x(ex + 1)
        wds = []
        for dg in range(4):
            di, wd = rot(wdn_t, "wdn")
            P.dma("pool", wd, wdn_d[ex][:, dg * 512:(dg + 1) * 512].rearrange("(k p) n -> p k n", p=128), writes=[f"wdn{di}"])
            wds.append((wd, f"wdn{di}"))
        for sbk in range(NSB):
            transpose_blocks(lambda c, sbk=sbk: xtile[:, sbk, c * 128:(c + 1) * 128], xk, 16,
                             lambda c, sbk=sbk: xT[:, c, sbk * 128:(sbk + 1) * 128], lambda c: "xT")
        for fg in range(2):
            (wg, wgk), (wu, wuk) = gu_q.pop(0)
            for fc in range(4):
                gi_, pg = rot(pA, "pA")
                for k in range(16):
                    P.add("pe", lambda e, pg=pg, wg=wg, k=k, fc=fc: e.matmul(pg[:, 0:CAP], lhsT=wg[:, k, fc * 128:(fc + 1) * 128], rhs=xT[:, k, :],
                                                                             start=(k == 0), stop=(k == 15)), reads=[wgk, "xT"], writes=[f"pA{gi_}"])
                ui_, pu = rot(pA, "pA")
                for k in range(16):
                    P.add("pe", lambda e, pu=pu, wu=wu, k=k, fc=fc: e.matmul(pu[:, 0:CAP], lhsT=wu[:, k, fc * 128:(fc + 1) * 128], rhs=xT[:, k, :],
                                                                             start=(k == 0), stop=(k == 15)), reads=[wuk, "xT"], writes=[f"pA{ui_}"])
                si_, gs = rot(gsb, "gsb")
                P.add("act", lambda e, gs=gs, pg=pg: e.activation(out=gs, in_=pg[:, 0:CAP], func=AF.Silu), reads=[f"pA{gi_}"], writes=[f"gsb{si_}"])
                P.add("dve", lambda e, gs=gs, pu=pu, fg=fg, fc=fc: e.tensor_tensor(out=actT[:, fg * 4 + fc, :], in0=pu[:, 0:CAP], in1=gs, op=ALU.mult),
                      reads=[f"pA{ui_}", f"gsb{si_}"], writes=["actT"])
            if ex + 1 < NE:
                gu_q.append(load_gu(ex + 1, fg))
        for sbk in range(NSB):
            yi, yt = rot(yst, "yst")
            for dg in range(4):
                wd, wdk = wds[dg]
                pi, pt = rot(pA, "pA")
                for k in range(8):
                    P.add("pe", lambda e, pt=pt, wd=wd, k=k, sbk=sbk: e.matmul(pt, lhsT=actT[:, k, sbk * 128:(sbk + 1) * 128], rhs=wd[:, k, :],
                                                                               start=(k == 0), stop=(k == 7)), reads=["actT", wdk], writes=[f"pA{pi}"])
                P.add("act", lambda e, pt=pt, yt=yt, dg=dg: e.copy(yt[:, dg * 512:(dg + 1) * 512], pt), reads=[f"pA{pi}"], writes=[f"yst{yi}"])
            P.dma("sp", ys_d[ex * CAP + sbk * 128:ex * CAP + (sbk + 1) * 128, :], yt, reads=[f"yst{yi}"], writes=["ys_d"])
    P.barrier()
    RF.reset(RF_mark)
    R0.reset()
    R1.reset()

    xt = [R0.alloc([128, D], F32) for _ in range(2)]
    y1 = [R0.alloc([128, D], BF16) for _ in range(2)]
    y2 = [R0.alloc([128, D], BF16) for _ in range(2)]
    ff = R0.alloc([128, D], F32)
    sq_junk = R0.alloc([128, D], BF16)
    P.dma("sp", bcX, mod_d.ap()[5 * D:6 * D].partition_broadcast(128), reads=["mod_d"], writes=["bcX"])
    P.dma("sp", bcY, gfin_d[:, :], writes=["bcY"])
    for blk in range(NB):
        xi, xtile = rot(xt, "xt")
        xk = f"xt{xi}"
        yi, y1t = rot(y1, "y1")
        _, y2t = rot(y2, "y2")
        P.dma("sp", xtile, x1_d[blk * 128:(blk + 1) * 128, :], reads=["x1_d"], writes=[xk])
        for kk, yt in ((0, y1t), (1, y2t)):
            yk = f"y{kk}_{yi}"
            P.add("pool", lambda e, yt=yt: e.memset(yt, 0.0), writes=[yk])
            P.add("pool", lambda e, yt=yt, blk=blk, kk=kk: e.indirect_dma_start(
                out=yt, out_offset=None, in_=ys_d[:, :], in_offset=bass.IndirectOffsetOnAxis(ap=dest_i[:, blk, kk:kk + 1], axis=0)),
                reads=["ys_d", "dest_i"], writes=[yk], dma=True)
        P.add("dve", lambda e, y1t=y1t, blk=blk: e.tensor_scalar(out=ff, in0=y1t, scalar1=wsel2[:, blk, 0:1], scalar2=None, op0=ALU.mult),
              reads=[f"y0_{yi}", "wsel2"], writes=["ff"])
        P.add("dve", lambda e, y2t=y2t, blk=blk: e.scalar_tensor_tensor(out=ff, in0=y2t, scalar=wsel2[:, blk, 1:2], in1=ff, op0=ALU.mult, op1=ALU.add),
              reads=[f"y1_{yi}", "wsel2", "ff"], writes=["ff"])
        P.add("pool", lambda e: e.tensor_tensor(out=ff, in0=ff, in1=bcX, op=ALU.mult), reads=["ff", "bcX"], writes=["ff"])
        P.add("pool", lambda e, xtile=xtile: e.tensor_tensor(out=xtile, in0=xtile, in1=ff, op=ALU.add), reads=[xk, "ff"], writes=[xk])
        rms_rstd(xtile, xk, 2, D, sq_junk, "sq_junk")
        P.add("dve", lambda e, xtile=xtile: e.scalar_tensor_tensor(out=xtile, in0=xtile, scalar=stats[:, 2:3], in1=bcY, op0=ALU.mult, op1=ALU.mult),
              reads=[xk, "stats2", "bcY"], writes=[xk])
        P.dma("sp", out_d[blk * 128:(blk + 1) * 128, :], xtile, reads=[xk], writes=["out_d"])
    P.barrier()
    P.emit()
    es.close()
    return nc


_NC_CACHE = {}


def make_in_maps(x, c, positions, w_ada, b_ada, g_mix, w_in, swa_sinks, diff_lambda_q1, diff_lambda_k1, diff_lambda_q2,
                 diff_lambda_k2, diff_subln_g, w_branch_a, w_branch_b, w_out, g_ffn, w_router_group, b_router_group,
                 w_router_expert, b_router_expert, w_exp_gate_up, w_exp_down, g_final):
    f32 = np.float32
    A = lambda a: np.ascontiguousarray(np.asarray(a))
    x = A(x); c = A(c); positions = A(positions)
    rep = lambda v: np.ascontiguousarray(np.broadcast_to(np.asarray(v, f32).reshape(1, -1), (128, np.asarray(v).size)))
    fm = lambda v: np.ascontiguousarray(np.asarray(v, f32).reshape(-1, 128).T)
    ident = np.eye(128, dtype=f32)
    p = np.arange(128)
    utri = (p[:, None] < p[None, :]).astype(f32)
    causal = (p[:, None] <= p[None, :]).astype(f32)
    swam = np.concatenate([causal, 1.0 - causal], axis=1)
    invf = (10000.0 ** (-np.arange(0, 64, 2, dtype=f32) / 64.0)).astype(f32)
    shared = {
        "w_ada": A(w_ada)[0], "b_ada": A(b_ada)[0].reshape(1, -1), "g_mixT": fm(A(g_mix)[0]), "w_in": A(w_in)[0],
        "sinks": rep(A(swa_sinks)[0]),
        "lamv": np.ascontiguousarray(np.broadcast_to(np.stack([A(diff_lambda_q1)[0], A(diff_lambda_k1)[0], A(diff_lambda_q2)[0],
                                                                A(diff_lambda_k2)[0]])[None], (128, 4, 64))).astype(f32),
        "subln": rep(A(diff_subln_g)[0]), "w_a": A(w_branch_a)[0], "w_b": A(w_branch_b)[0], "w_o": A(w_out)[0],
        "g_ffn": rep(A(g_ffn)[0]),
        "w_r": np.ascontiguousarray(np.concatenate([A(w_router_group)[0], A(w_router_expert)[0]], axis=1)),
        "b_r": rep(np.concatenate([A(b_router_group)[0], A(b_router_expert)[0]])),
        "w_gu": A(w_exp_gate_up)[0], "w_dn": A(w_exp_down)[0], "g_fin": rep(A(g_final)),
        "ident_b": ident.astype(ml_dtypes.bfloat16), "ident_f": ident,
        "utri": utri, "causal": causal.astype(ml_dtypes.bfloat16), "swamask": swam.astype(ml_dtypes.bfloat16),
        "invfreq": rep((invf.astype(np.float64) / (2.0 * np.pi)).astype(f32)),
        "eidx": rep(np.arange(NE, dtype=f32)),
    }
    in_maps = []
    for b in range(NCORES):
        m = dict(shared)
        m["x"] = x[b]
        m["cT"] = fm(c[b])
        m["pos"] = np.ascontiguousarray(positions[b].reshape(NB, 128).T.astype(np.int32))
        in_maps.append(m)
    return in_maps


def kernel(**inputs):
    in_maps = make_in_maps(**inputs)
    if "nc" not in _NC_CACHE:
        _NC_CACHE["nc"] = build_program()
    res = run_bass_kernel_spmd(_NC_CACHE["nc"], in_maps, core_ids=list(range(NCORES)))
    return np.stack([np.asarray(res.results[b]["out"], dtype=np.float32) for b in range(NCORES)], axis=0)
```
